# Optimizing a Trainium2 kernel written in Bass

```python
import math
import jax, jax.numpy as jnp
from jax import lax
import numpy as np

D_MODEL = 2048
BATCH = 4
SEQ = 8192
DEPTH = 2

N_BRANCHES = 3
BRANCH_WIDTH = D_MODEL // 2
MLSTM_HEADS = 4
MLSTM_DV = BRANCH_WIDTH // MLSTM_HEADS
MLSTM_DQK = MLSTM_DV // 2
MLSTM_CHUNK = 64
CONV_WIDTH = 4
NSA_HEADS = 8
NSA_KV_GROUPS = 2
NSA_HPG = NSA_HEADS // NSA_KV_GROUPS
NSA_DH = BRANCH_WIDTH // NSA_HEADS
CMP_LEN = 32
CMP_STRIDE = 16
SLC_LEN = 64
NSA_N_SELECT = 16
WINDOW = 512
Q_BLOCK = 128
NUM_BUCKETS = 32
MAX_DISTANCE = 1024
GMLP_CHUNK = 128
GMLP_GROUPS = 8
GMLP_GW = BRANCH_WIDTH // GMLP_GROUPS
N_EXPERTS = 32
TOP_K = 4
D_FF = 3 * D_MODEL // 4
SWIGLU_LIMIT = 7.0
SWIGLU_ALPHA = 1.702
EXPERT_BLOCK = 256
NEG_INF = -1e30
FORCE_SELECT = 1e4
IN_SIZES = (
    MLSTM_HEADS * MLSTM_DQK, MLSTM_HEADS * MLSTM_DQK, MLSTM_HEADS * MLSTM_DV,
    MLSTM_HEADS, MLSTM_HEADS, BRANCH_WIDTH,
    NSA_HEADS * NSA_DH,
    NSA_KV_GROUPS * NSA_DH, NSA_KV_GROUPS * NSA_DH,
    NSA_KV_GROUPS * NSA_DH, NSA_KV_GROUPS * NSA_DH,
    NSA_KV_GROUPS * NSA_DH, NSA_KV_GROUPS * NSA_DH,
    NSA_HEADS * 3,
    BRANCH_WIDTH, BRANCH_WIDTH,
)
IN_TOTAL = sum(IN_SIZES)

kernel_name = 'hybrid_mlstm_nsa_gmlp_moe_block'


def rms_norm(x, g, eps=1e-6):
    xf = x.astype(jnp.float32)
    y = xf * lax.rsqrt(jnp.mean(xf * xf, axis=-1, keepdims=True) + eps)
    return (y * g.astype(jnp.float32)).astype(x.dtype)


def layer_norm(x, g, eps=1e-6):
    xf = x.astype(jnp.float32)
    mu = jnp.mean(xf, axis=-1, keepdims=True)
    var = jnp.mean(jnp.square(xf - mu), axis=-1, keepdims=True)
    return ((xf - mu) * lax.rsqrt(var + eps) * g.astype(jnp.float32)).astype(x.dtype)


def masked_softmax(s, mask):
    p = jax.nn.softmax(jnp.where(mask, s.astype(jnp.float32), NEG_INF), axis=-1)
    return jnp.where(mask, p, 0.0)


def t5_bucket(dist):
    dist = jnp.maximum(dist, 0)
    max_exact = NUM_BUCKETS // 2
    log_ratio = jnp.log(jnp.maximum(dist, 1).astype(jnp.float32) / max_exact) / math.log(MAX_DISTANCE / max_exact)
    large = jnp.minimum(max_exact + (log_ratio * (NUM_BUCKETS - max_exact)).astype(jnp.int32), NUM_BUCKETS - 1)
    return jnp.where(dist < max_exact, dist, large)


def causal_conv(x, w, b):
    S = x.shape[1]
    K = w.shape[0]
    xp = jnp.pad(x, ((0, 0), (K - 1, 0), (0, 0)))
    y = b
    for j in range(K):
        y = y + w[j] * xp[:, j:j + S]
    return y


def mlstm_chunkwise(q, k, v, logi, logf):
    B, S, H, DQK = q.shape
    DV = v.shape[-1]
    L = MLSTM_CHUNK
    NCH = S // L

    def to_chunks(t):
        return t.reshape((B, NCH, L) + t.shape[2:]).swapaxes(0, 1)

    xs = tuple(to_chunks(t) for t in (q, k, v, logi, logf))
    causal = jnp.tril(jnp.ones((L, L), dtype=bool))[None, :, :, None]

    def step(carry, xs_c):
        C, n, m = carry
        qb, kb, vb, ib, fb = xs_c
        b = jnp.cumsum(fb, axis=1)
        a = b + m[:, None, :]
        D = jnp.where(causal, b[:, :, None, :] - b[:, None, :, :] + ib[:, None, :, :], -jnp.inf)
        m_t = jnp.maximum(a, D.max(axis=2))
        w_intra = jnp.exp(D - m_t[:, :, None, :])
        w_inter = jnp.exp(a - m_t)
        qk = jnp.einsum('bthd,bshd->btsh', qb, kb) * w_intra
        num = jnp.einsum('btsh,bshv->bthv', qk, vb) + w_inter[..., None] * jnp.einsum('bhvd,bthd->bthv', C, qb)
        den = qk.sum(axis=2) + w_inter * jnp.einsum('bhd,bthd->bth', n, qb)
        h = num / jnp.maximum(jnp.abs(den), jnp.exp(-m_t))[..., None]
        bL = b[:, -1, :]
        g = bL[:, None, :] - b + ib
        m_new = jnp.maximum(bL + m, g.max(axis=1))
        ws = jnp.exp(g - m_new[:, None, :])
        wc = jnp.exp(bL + m - m_new)
        C = wc[..., None, None] * C + jnp.einsum('bsh,bshv,bshd->bhvd', ws, vb, kb)
        n = wc[..., None] * n + jnp.einsum('bsh,bshd->bhd', ws, kb)
        return (C, n, m_new), h

    init = (jnp.zeros((B, H, DV, DQK), jnp.float32), jnp.zeros((B, H, DQK), jnp.float32),
            jnp.full((B, H), NEG_INF, jnp.float32))
    _, hs = lax.scan(step, init, xs)
    return hs.swapaxes(0, 1).reshape(B, S, H, DV)


def nsa_attention(q, k_c, v_c, k_s, v_s, k_w, v_w, gates, cmp_pos, cmp_k_w1, cmp_k_w2,
                  cmp_v_w1, cmp_v_w2, qnorm_g, knorm_g, rel_bias):
    B, S = q.shape[:2]
    G, HPG, DH = NSA_KV_GROUPS, NSA_HPG, NSA_DH
    f32 = jnp.float32
    q = (rms_norm(q.reshape(B, S, G, HPG, DH), qnorm_g) * DH ** -0.5).astype(f32)

    NC = (S - CMP_LEN) // CMP_STRIDE + 1
    cidx = jnp.arange(NC)[:, None] * CMP_STRIDE + jnp.arange(CMP_LEN)[None, :]

    def compress(t, w1, w2):
        blk = t.reshape(B, S, G, DH)[:, cidx] + cmp_pos[None, None, :, None, :]
        flat = blk.transpose(0, 1, 3, 2, 4).reshape(B, NC, G, CMP_LEN * DH)
        return jax.nn.gelu(flat @ w1) @ w2

    k_cmp = rms_norm(compress(k_c, cmp_k_w1, cmp_k_w2), knorm_g).astype(f32)
    v_cmp = compress(v_c, cmp_v_w1, cmp_v_w2).astype(f32)
    cstart = jnp.arange(NC) * CMP_STRIDE
    cend = cstart + CMP_LEN - 1

    NS = S // SLC_LEN
    n_sel = min(NSA_N_SELECT, NS)
    ks_blk = rms_norm(k_s.reshape(B, S, G, DH), knorm_g).astype(f32).reshape(B, NS, SLC_LEN, G, DH).transpose(0, 3, 1, 2, 4)
    vs_blk = v_s.astype(f32).reshape(B, NS, SLC_LEN, G, DH).transpose(0, 3, 1, 2, 4)
    sstart = jnp.arange(NS) * SLC_LEN
    overlap = jnp.clip(jnp.minimum(cstart[:, None] + CMP_LEN, sstart[None, :] + SLC_LEN)
                       - jnp.maximum(cstart[:, None], sstart[None, :]), 0, None).astype(f32) / CMP_LEN

    pad = ((0, 0), (WINDOW, 0), (0, 0), (0, 0))
    kw_pad = jnp.pad(rms_norm(k_w.reshape(B, S, G, DH), knorm_g).astype(f32), pad)
    vw_pad = jnp.pad(v_w.reshape(B, S, G, DH).astype(f32), pad)

    rb = rel_bias.astype(f32).reshape(NUM_BUCKETS, G, HPG)
    g = jax.nn.sigmoid(gates.astype(f32)).reshape(B, S, G, HPG, 3)
    NQB = S // Q_BLOCK
    q_blocks = q.reshape(B, NQB, Q_BLOCK, G, HPG, DH).swapaxes(0, 1)
    g_blocks = g.reshape(B, NQB, Q_BLOCK, G, HPG, 3).swapaxes(0, 1)
    bi = jnp.arange(B)[:, None, None, None]
    gi = jnp.arange(G)[None, :, None, None]
    jn = jnp.arange(NS)

    def one_block(args):
        qb, gb, j = args
        start = j * Q_BLOCK
        tpos = start + jnp.arange(Q_BLOCK)
        s_c = jnp.einsum('bqghd,bcgd->bghqc', qb, k_cmp)
        s_c = s_c + rb[t5_bucket(tpos[:, None] - cend[None, :])].transpose(2, 3, 0, 1)
        p_c = masked_softmax(s_c, cend[None, :] <= tpos[:, None])
        o_c = jnp.einsum('bghqc,bcgd->bqghd', p_c, v_cmp)
        imp = jnp.einsum('bghqc,cn->bgqn', p_c, overlap)
        blk_t = tpos // SLC_LEN
        valid = sstart[None, :] <= tpos[:, None]
        forced = (jn[None, :] == 0) | (jn[None, :] == blk_t[:, None]) | (jn[None, :] == blk_t[:, None] - 1)
        score = jnp.where(valid, imp + jnp.where(forced, FORCE_SELECT, 0.0), NEG_INF)
        _, sel = lax.top_k(score, n_sel)
        k_sel = ks_blk[bi, gi, sel].reshape(B, G, Q_BLOCK, n_sel * SLC_LEN, DH)
        v_sel = vs_blk[bi, gi, sel].reshape(B, G, Q_BLOCK, n_sel * SLC_LEN, DH)
        kpos = (sel[..., None] * SLC_LEN + jnp.arange(SLC_LEN)).reshape(B, G, Q_BLOCK, n_sel * SLC_LEN)
        dist_s = tpos[None, None, :, None] - kpos
        s_s = jnp.einsum('bqghd,bgqkd->bghqk', qb, k_sel) + rb[t5_bucket(dist_s), gi].transpose(0, 1, 4, 2, 3)
        p_s = masked_softmax(s_s, (dist_s >= 0)[:, :, None])
        o_s = jnp.einsum('bghqk,bgqkd->bqghd', p_s, v_sel)
        kwin = lax.dynamic_slice_in_dim(kw_pad, start, Q_BLOCK + WINDOW, axis=1)
        vwin = lax.dynamic_slice_in_dim(vw_pad, start, Q_BLOCK + WINDOW, axis=1)
        wpos = start - WINDOW + jnp.arange(Q_BLOCK + WINDOW)
        dist_w = tpos[:, None] - wpos[None, :]
        mask_w = (dist_w >= 0) & (dist_w < WINDOW) & (wpos[None, :] >= 0)
        s_w = jnp.einsum('bqghd,bkgd->bghqk', qb, kwin) + rb[t5_bucket(dist_w)].transpose(2, 3, 0, 1)
        p_w = masked_softmax(s_w, mask_w)
        o_w = jnp.einsum('bghqk,bkgd->bqghd', p_w, vwin)
        return gb[..., 0:1] * o_c + gb[..., 1:2] * o_s + gb[..., 2:3] * o_w

    out = lax.map(one_block, (q_blocks, g_blocks, jnp.arange(NQB)))
    return out.swapaxes(0, 1).reshape(B, S, NSA_HEADS * DH)


def gmlp_spatial(u, v, norm_g, ws, b):
    B, S, _ = u.shape
    u = jax.nn.gelu(u)
    v = layer_norm(jax.nn.gelu(v), norm_g)
    vc = v.reshape(B, S // GMLP_CHUNK, GMLP_CHUNK, GMLP_GROUPS, GMLP_GW)
    ws_causal = ws * jnp.tril(jnp.ones((GMLP_CHUNK, GMLP_CHUNK), ws.dtype))
    s = jnp.einsum('gts,bnsgd->bntgd', ws_causal, vc) + b.T[None, None, :, :, None]
    return u * s.reshape(B, S, BRANCH_WIDTH)


def token_mixers(h, w_in, conv_w, conv_b, mlstm_gate_b, mlstm_norm_g, cmp_pos, cmp_k_w1, cmp_k_w2,
                 cmp_v_w1, cmp_v_w2, qnorm_g, knorm_g, rel_bias, gmlp_norm_g, gmlp_ws, gmlp_b,
                 w_branch, w_gate, w_out):
    B, S, _ = h.shape
    z = h @ w_in
    points = [int(p) for p in np.cumsum(IN_SIZES)[:-1]]
    (qm, km, vm, ig, fg, og, qn, kc, vc, ks, vs, kw, vw, gn, gu, gv) = jnp.split(z, points, axis=-1)

    qk = jax.nn.silu(causal_conv(jnp.concatenate([qm, km], axis=-1), conv_w, conv_b)).astype(jnp.float32)
    qm, km = jnp.split(qk, 2, axis=-1)
    q = qm.reshape(B, S, MLSTM_HEADS, MLSTM_DQK) * MLSTM_DQK ** -0.5
    k = km.reshape(B, S, MLSTM_HEADS, MLSTM_DQK)
    v = vm.astype(jnp.float32).reshape(B, S, MLSTM_HEADS, MLSTM_DV)
    logi = (ig + mlstm_gate_b[:MLSTM_HEADS]).astype(jnp.float32)
    logf = jax.nn.log_sigmoid((fg + mlstm_gate_b[MLSTM_HEADS:]).astype(jnp.float32))
    hm = mlstm_chunkwise(q, k, v, logi, logf)
    hm = rms_norm(hm, mlstm_norm_g.reshape(MLSTM_HEADS, MLSTM_DV)).reshape(B, S, BRANCH_WIDTH)
    y_a = (jax.nn.sigmoid(og.astype(jnp.float32)) * hm).astype(h.dtype)

    y_b = nsa_attention(qn, kc, vc, ks, vs, kw, vw, gn, cmp_pos, cmp_k_w1, cmp_k_w2, cmp_v_w1, cmp_v_w2,
                        qnorm_g, knorm_g, rel_bias).astype(h.dtype)

    y_c = gmlp_spatial(gu, gv, gmlp_norm_g, gmlp_ws, gmlp_b)

    merged = 0.0
    for i, y in enumerate((y_a, y_b, y_c)):
        merged = merged + jax.nn.sigmoid(h @ w_gate[i]) * (y @ w_branch[i])
    return merged @ w_out


def moe_ffn(h, router_w, router_b, w1, b1, w2, b2):
    B, S, D = h.shape
    N = B * S
    t = h.reshape(N, D)
    logits = (t @ router_w + router_b).astype(jnp.float32)
    top_val, top_idx = lax.top_k(logits, TOP_K)
    probs = jax.nn.softmax(top_val, axis=-1)
    A = N * TOP_K
    e_flat = top_idx.reshape(A)
    order = jnp.argsort(e_flat)
    e_sorted = e_flat[order]
    tok_sorted = order // TOP_K
    counts = jnp.bincount(e_flat, length=N_EXPERTS)
    padded = (counts + EXPERT_BLOCK - 1) // EXPERT_BLOCK * EXPERT_BLOCK
    pend = jnp.cumsum(padded)
    pstart = pend - padded
    cstart = jnp.cumsum(counts) - counts
    dest = pstart[e_sorted] + (jnp.arange(A) - cstart[e_sorted])
    NB = A // EXPERT_BLOCK + N_EXPERTS
    buf_tok = jnp.zeros((NB * EXPERT_BLOCK,), jnp.int32).at[dest].set(tok_sorted)
    block_exp = jnp.minimum(jnp.searchsorted(pend, jnp.arange(NB) * EXPERT_BLOCK, side='right'), N_EXPERTS - 1)
    xb = t[buf_tok].reshape(NB, EXPERT_BLOCK, D)

    def expert_block(args):
        xblk, e = args
        gu = xblk @ w1[e] + b1[e]
        gate, up = jnp.split(gu, 2, axis=-1)
        gate = jnp.minimum(gate, SWIGLU_LIMIT)
        up = jnp.clip(up, -SWIGLU_LIMIT, SWIGLU_LIMIT)
        act = (up + 1.0) * (gate * jax.nn.sigmoid(SWIGLU_ALPHA * gate))
        return act @ w2[e] + b2[e]

    yb = lax.map(expert_block, (xb, block_exp)).reshape(NB * EXPERT_BLOCK, D)
    y_sorted = yb[dest] * probs.reshape(A)[order][:, None].astype(yb.dtype)
    return jax.ops.segment_sum(y_sorted, tok_sorted, num_segments=N).reshape(B, S, D)


def setup_inputs(seed: int = 0) -> dict:
    key = jax.random.key(seed)
    ks = iter(jax.random.split(key, 40))
    L, D, BW, DH, E, F = DEPTH, D_MODEL, BRANCH_WIDTH, NSA_DH, N_EXPERTS, D_FF

    def nrm(shape, scale):
        return jax.random.normal(next(ks), shape, jnp.float32) * scale

    def gain(shape):
        return 1.0 + nrm(shape, 0.02)

    return {
        'x': nrm((BATCH, SEQ, D), 1.0),
        'c': nrm((BATCH, D), 1.0),
        'ada_w': nrm((L, D, 6 * D), 0.5 * D ** -0.5),
        'ada_b': nrm((L, 6 * D), 0.02),
        'norm1_g': gain((L, D)),
        'norm2_g': gain((L, D)),
        'w_in': nrm((L, D, IN_TOTAL), D ** -0.5),
        'conv_w': nrm((L, CONV_WIDTH, 2 * MLSTM_HEADS * MLSTM_DQK), CONV_WIDTH ** -0.5),
        'conv_b': nrm((L, 2 * MLSTM_HEADS * MLSTM_DQK), 0.02),
        'mlstm_gate_b': jnp.concatenate([nrm((L, MLSTM_HEADS), 0.1), 3.0 + nrm((L, MLSTM_HEADS), 0.5)], axis=-1),
        'mlstm_norm_g': gain((L, BW)),
        'cmp_pos': nrm((L, CMP_LEN, DH), 0.1),
        'cmp_k_w1': nrm((L, CMP_LEN * DH, DH), (CMP_LEN * DH) ** -0.5),
        'cmp_k_w2': nrm((L, DH, DH), DH ** -0.5),
        'cmp_v_w1': nrm((L, CMP_LEN * DH, DH), (CMP_LEN * DH) ** -0.5),
        'cmp_v_w2': nrm((L, DH, DH), DH ** -0.5),
        'qnorm_g': gain((L, DH)),
        'knorm_g': gain((L, DH)),
        'rel_bias': nrm((NUM_BUCKETS, NSA_HEADS), 0.5),
        'gmlp_norm_g': gain((L, BW)),
        'gmlp_ws': nrm((L, GMLP_GROUPS, GMLP_CHUNK, GMLP_CHUNK), GMLP_CHUNK ** -0.5),
        'gmlp_b': 1.0 + nrm((L, GMLP_GROUPS, GMLP_CHUNK), 0.02),
        'w_branch': nrm((L, N_BRANCHES, BW, D), BW ** -0.5),
        'w_gate': nrm((L, N_BRANCHES, D, D), D ** -0.5),
        'w_out': nrm((L, D, D), D ** -0.5),
        'router_w': nrm((L, D, E), D ** -0.5),
        'router_b': nrm((L, E), 0.01),
        'exp_w1': nrm((L, E, D, 2 * F), D ** -0.5),
        'exp_b1': nrm((L, E, 2 * F), 0.02),
        'exp_w2': nrm((L, E, F, D), F ** -0.5),
        'exp_b2': nrm((L, E, D), 0.02),
    }


def reference(x, c, ada_w, ada_b, norm1_g, norm2_g, w_in, conv_w, conv_b, mlstm_gate_b, mlstm_norm_g,
              cmp_pos, cmp_k_w1, cmp_k_w2, cmp_v_w1, cmp_v_w2, qnorm_g, knorm_g, rel_bias,
              gmlp_norm_g, gmlp_ws, gmlp_b, w_branch, w_gate, w_out, router_w, router_b,
              exp_w1, exp_b1, exp_w2, exp_b2):
    for l in range(DEPTH):
        mod = jax.nn.silu(c) @ ada_w[l] + ada_b[l]
        sh1, sc1, g1, sh2, sc2, g2 = [m[:, None, :] for m in jnp.split(mod, 6, axis=-1)]
        h = rms_norm(x, norm1_g[l]) * (1.0 + sc1) + sh1
        x = x + g1 * token_mixers(h, w_in[l], conv_w[l], conv_b[l], mlstm_gate_b[l], mlstm_norm_g[l],
                                  cmp_pos[l], cmp_k_w1[l], cmp_k_w2[l], cmp_v_w1[l], cmp_v_w2[l],
                                  qnorm_g[l], knorm_g[l], rel_bias, gmlp_norm_g[l], gmlp_ws[l], gmlp_b[l],
                                  w_branch[l], w_gate[l], w_out[l])
        h = rms_norm(x, norm2_g[l]) * (1.0 + sc2) + sh2
        x = x + g2 * moe_ffn(h, router_w[l], router_b[l], exp_w1[l], exp_b1[l], exp_w2[l], exp_b2[l])
    return x
```

```python
import contextlib
import numpy as np
import concourse.bass as bass
import concourse.mybir as mybir
from concourse.bass_utils import run_bass_kernel_spmd

F32 = mybir.dt.float32
BF16 = mybir.dt.bfloat16
I32 = mybir.dt.int32
ALU = mybir.AluOpType
AF = mybir.ActivationFunctionType
AX = mybir.AxisListType

COMPUTE = ("pe", "dve", "act", "pool")
ALLENG = ("pe", "dve", "act", "pool", "sp")


class _Op:
    __slots__ = ("eng", "fn", "deps", "dma", "idx", "ms", "dsem", "dval", "dprev", "signal")


class Prog:
    RING = 8

    def __init__(self, nc, st):
        self.nc = nc
        self.ops = []
        self.lw = {}
        self.rd = {}
        self.csem = {e: st.enter_context(nc.semaphore("s_" + e)) for e in COMPUTE}
        self.dsem = {}
        for e in ("sp", "act", "pool"):
            for r in range(self.RING):
                self.dsem[(e, r)] = st.enter_context(nc.semaphore("d_%s_%d" % (e, r)))
        self.bar = st.enter_context(nc.semaphore("s_bar"))
        self.cnt = {e: 0 for e in COMPUTE}
        self.dcnt = {e: 0 for e in ("sp", "act", "pool")}
        self.waited = {e: {} for e in ALLENG}
        self.nflush = 0
        self.base = 0

    def op(self, eng, fn, reads=(), writes=(), dma=False):
        idx = self.base + len(self.ops)
        deps = set()
        for k in reads:
            w = self.lw.get(k)
            if w is not None:
                deps.add(w)
        for k in writes:
            w = self.lw.get(k)
            if w is not None:
                deps.add(w)
            for r in self.rd.get(k, ()):
                deps.add(r)
        o = _Op()
        o.eng, o.fn, o.deps, o.dma, o.idx, o.ms, o.signal = eng, fn, deps, dma, idx, None, False
        self.ops.append(o)
        for k in reads:
            self.rd.setdefault(k, []).append(idx)
        for k in writes:
            self.lw[k] = idx
            self.rd[k] = []
        return idx

    def dma(self, eng, out, in_, reads=(), writes=(), **kw):
        return self.op(eng, lambda e: e.dma_start(out=out, in_=in_, **kw), reads, writes, dma=True)

    def flush(self):
        nc = self.nc
        ops = self.ops
        base = self.base
        last_compute = {}
        for o in ops:
            if not o.dma:
                last_compute[o.eng] = o
            for d in o.deps:
                if d < base:
                    continue
                p = ops[d - base]
                if p.dma:
                    continue
                if p.eng == "pe" and o.eng == "pe" and not o.dma:
                    continue
                p.signal = True
        for o in last_compute.values():
            o.signal = True
        for o in ops:
            if o.dma:
                k = self.dcnt[o.eng]
                self.dcnt[o.eng] = k + 1
                o.dsem = (o.eng, k % self.RING)
                o.dval = 16 * (k // self.RING + 1)
                o.dprev = 16 * (k // self.RING)
            elif o.signal:
                self.cnt[o.eng] += 1
                o.ms = self.cnt[o.eng]
        self.nflush += 1
        nfl = self.nflush
        final_cnt = dict(self.cnt)
        final_d = {}
        for e in self.dcnt:
            k = self.dcnt[e]
            for r in range(self.RING):
                n = (k - r + self.RING - 1) // self.RING if k > r else 0
                final_d[(e, r)] = 16 * n

        def run_engine(ename, eobj):
            waited = self.waited[ename]

            def wait(key, sem, val):
                if val <= 0 or waited.get(key, 0) >= val:
                    return
                waited[key] = val
                eobj.wait_ge(sem, val)

            for o in ops:
                if o.eng != ename:
                    continue
                for d in sorted(o.deps):
                    if d < base:
                        continue
                    p = ops[d - base]
                    if p.dma:
                        wait(p.dsem, self.dsem[p.dsem], p.dval)
                    else:
                        if p.eng == "pe" and o.eng == "pe" and not o.dma:
                            continue
                        wait(p.eng, self.csem[p.eng], p.ms)
                if o.dma:
                    wait(o.dsem, self.dsem[o.dsem], o.dprev)
                    ins = o.fn(eobj)
                    ins.then_inc(self.dsem[o.dsem], 16)
                else:
                    ins = o.fn(eobj)
                    if o.signal:
                        ins.then_inc(self.csem[o.eng], 1)
            if ename in self.dcnt:
                for r in range(self.RING):
                    wait((ename, r), self.dsem[(ename, r)], final_d[(ename, r)])
            if ename in final_cnt:
                wait(ename, self.csem[ename], final_cnt[ename])
            eobj.sem_inc(self.bar, 1)
            eobj.wait_ge(self.bar, 5 * nfl)

        with nc.Block() as block:
            emap = {"pe": block.tensor, "dve": block.vector, "act": block.scalar,
                    "pool": block.gpsimd, "sp": block.sync}
            for e in ALLENG:
                def mk(e=e):
                    def f(eobj):
                        run_engine(e, eobj)
                    return f
                emap[e](mk())
        self.base += len(ops)
        self.ops = []
        self.lw = {}
        self.rd = {}


D = 2048
NB = 4
SEQ = 8192
DEPTH = 2
IN_TOTAL = 7712
OFF = dict(qm=0, km=512, vm=1024, ig=2048, fg=2052, og=2056, qn=3080, kc=4104, vc=4360, ks=4616,
           vs=4872, kw=5128, vw=5384, gn=5640, gu=5664, gv=6688)
EPS = 1e-6


def _new_nc():
    return bass.Bass("TRN2", target_bir_lowering=False)


def build_ada():
    nc = _new_nc()
    ct = nc.dram_tensor("ct", [128, 16, NB], F32, kind="ExternalInput").ap()
    aw = nc.dram_tensor("aw", [DEPTH, 128, 16, 1536], F32, kind="ExternalInput").ap()
    ab = nc.dram_tensor("ab", [DEPTH, NB, 1536], F32, kind="ExternalInput").ap()
    out = nc.dram_tensor("mod", [DEPTH, NB, 1536], F32, kind="ExternalOutput").ap()
    with contextlib.ExitStack() as st:
        P = Prog(nc, st)
        cts = st.enter_context(nc.sbuf_tensor("cts", [128, 16, NB], F32))
        sc = st.enter_context(nc.sbuf_tensor("sc", [128, 16, NB], F32))
        wt = st.enter_context(nc.sbuf_tensor("wt", [128, 2, 16, 512], F32))
        bt = st.enter_context(nc.sbuf_tensor("bt", [NB, DEPTH, 1536], F32))
        ot = st.enter_context(nc.sbuf_tensor("ot", [NB, DEPTH, 1536], F32))
        ps = [st.enter_context(nc.psum_tensor("ps%d" % i, [128, 512], F32)) for i in range(2)]
        P.dma("sp", cts[:], ct, writes=["cts"])
        for l in range(DEPTH):
            P.dma("sp", bt[:, l, :], ab[l], writes=[("bt", l)])
        P.op("act", lambda e: e.activation(out=sc[:], in_=cts[:], func=AF.Silu), reads=["cts"], writes=["sc"])
        i = 0
        for l in range(DEPTH):
            for j in range(3):
                b = i % 2
                i += 1
                P.dma("sp", wt[:, b, :, :], aw[l, :, :, j * 512:(j + 1) * 512], writes=[("wt", b)])
                for k in range(16):
                    P.op("pe", lambda e, b=b, k=k: e.matmul(ps[b][0:NB, :], lhsT=sc[:, k, :], rhs=wt[:, b, k, :],
                                                          start=(k == 0), stop=(k == 15)),
                         reads=["sc", ("wt", b)], writes=[("ps", b)])
                P.op("dve", lambda e, b=b, l=l, j=j: e.tensor_tensor(out=ot[:, l, j * 512:(j + 1) * 512], in0=ps[b][0:NB, :],
                                                                   in1=bt[:, l, j * 512:(j + 1) * 512], op=ALU.add),
                     reads=[("ps", b), ("bt", l)], writes=[("ot", l, j)])
        for l in range(DEPTH):
            P.dma("sp", out[l], ot[:, l, :], reads=[("ot", l, j) for j in range(3)], writes=[("out", l)])
        P.flush()
    return nc


def run_ada(c, ada_w, ada_b):
    nc = build_ada()
    ct = np.ascontiguousarray(c.reshape(NB, 16, 128).transpose(2, 1, 0))
    in_maps = []
    for core in range(8):
        cols = slice(core * 1536, (core + 1) * 1536)
        aw = np.ascontiguousarray(ada_w[:, :, cols].reshape(DEPTH, 16, 128, 1536).transpose(0, 2, 1, 3))
        ab = np.ascontiguousarray(np.broadcast_to(ada_b[:, None, cols], (DEPTH, NB, 1536)))
        in_maps.append({"ct": ct, "aw": aw, "ab": ab})
    res = run_bass_kernel_spmd(nc, in_maps, core_ids=list(range(8)))
    mod = np.concatenate([r["mod"] for r in res.results], axis=2)
    return mod


NFM = 1536
NTM = 1308
FM_MQ, FM_MK, FM_NQ, FM_KC, FM_KS, FM_KW, FM_VC = 0, 256, 512, 1024, 1152, 1280, 1408
TM_MV, TM_OG, TM_VS, TM_VW, TM_GN, TM_IG, TM_FG = 0, 512, 1024, 1152, 1280, 1292, 1294


def la_columns(hh):
    fm = np.concatenate([
        np.arange(OFF["qm"] + hh * 256, OFF["qm"] + (hh + 1) * 256),
        np.arange(OFF["km"] + hh * 256, OFF["km"] + (hh + 1) * 256),
        np.arange(OFF["qn"] + hh * 512, OFF["qn"] + (hh + 1) * 512),
        np.arange(OFF["kc"] + hh * 128, OFF["kc"] + (hh + 1) * 128),
        np.arange(OFF["ks"] + hh * 128, OFF["ks"] + (hh + 1) * 128),
        np.arange(OFF["kw"] + hh * 128, OFF["kw"] + (hh + 1) * 128),
        np.arange(OFF["vc"] + hh * 128, OFF["vc"] + (hh + 1) * 128)])
    tm = np.concatenate([
        np.arange(OFF["vm"] + hh * 512, OFF["vm"] + (hh + 1) * 512),
        np.arange(OFF["og"] + hh * 512, OFF["og"] + (hh + 1) * 512),
        np.arange(OFF["vs"] + hh * 128, OFF["vs"] + (hh + 1) * 128),
        np.arange(OFF["vw"] + hh * 128, OFF["vw"] + (hh + 1) * 128),
        np.arange(OFF["gn"] + hh * 12, OFF["gn"] + (hh + 1) * 12),
        np.arange(OFF["ig"] + hh * 2, OFF["ig"] + (hh + 1) * 2),
        np.arange(OFF["fg"] + hh * 2, OFF["fg"] + (hh + 1) * 2),
        np.zeros(12, np.int64)])
    assert len(fm) == NFM and len(tm) == NTM
    return fm, tm


def emit_norm_to_hT(nc, P, st, x, S, modv, gv, ident, epst, hT_d, tagp="n1", xn_d=None, hook=None, xkey=None, dbl=True):
    xt = [st.enter_context(nc.sbuf_tensor("%s_xt%d" % (tagp, i), [128, D], F32)) for i in range(2)]
    xn = [st.enter_context(nc.sbuf_tensor("%s_xn%d" % (tagp, i), [128, D], F32)) for i in range(2)]
    ht = [st.enter_context(nc.sbuf_tensor("%s_ht%d" % (tagp, i), [128, 16, 512], BF16)) for i in range(2)]
    stat = st.enter_context(nc.sbuf_tensor(tagp + "_stat", [128, 4], F32))
    scl = st.enter_context(nc.sbuf_tensor(tagp + "_scl", [128, 16], F32))
    ps = [st.enter_context(nc.psum_tensor("%s_ps%d" % (tagp, i), [128, 512], F32)) for i in range(8 if dbl else 4)]
    P.op("dve", lambda e: e.scalar_tensor_tensor(out=scl[:], in0=modv[:, 1, :], scalar=1.0, in1=gv[:], op0=ALU.add, op1=ALU.mult),
         reads=["modv", "gv"], writes=[tagp + "scl"])
    it = 0
    for t in range(S // 512):
        hb = t % 2
        for sub in range(4):
            b = it % 2
            it += 1
            r0 = t * 512 + sub * 128
            P.dma("sp", xt[b][:], x[r0:r0 + 128, :], reads=([xkey] if xkey else []), writes=[(tagp + "xt", b)])
            P.op("act", lambda e, b=b: e.activation(out=xn[b][:], in_=xt[b][:], func=AF.Square, scale=float(D) ** -0.5,
                                                   accum_out=stat[:, b:b + 1]),
                 reads=[(tagp + "xt", b)], writes=[(tagp + "xn", b), (tagp + "st", b)])
            P.op("act", lambda e, b=b: e.activation(out=stat[:, b:b + 1], in_=stat[:, b:b + 1], func=AF.Sqrt, bias=epst[:, 0:1]),
                 reads=[(tagp + "st", b), "epst"], writes=[(tagp + "st", b)])
            P.op("dve", lambda e, b=b: e.reciprocal(out=stat[:, 2 + b:3 + b], in_=stat[:, b:b + 1]),
                 reads=[(tagp + "st", b)], writes=[(tagp + "rs", b)])
            P.op("act", lambda e, b=b: e.activation(out=xn[b][:], in_=xt[b][:], func=AF.Copy, scale=stat[:, 2 + b:3 + b]),
                 reads=[(tagp + "xt", b), (tagp + "rs", b)], writes=[(tagp + "xn", b)])
            if xn_d is not None:
                P.dma("act", xn_d[r0:r0 + 128, :], xn[b][:], reads=[(tagp + "xn", b)], writes=[("xn_d", r0)])
            for k in range(16):
                pb = ((b * 4 if dbl else 0) + k // 4)
                P.op("pe", lambda e, b=b, k=k, pb=pb: e.transpose(out=ps[pb][:, (k % 4) * 128:(k % 4 + 1) * 128],
                                                                in_=xn[b][:, k * 128:(k + 1) * 128], identity=ident[:]),
                     reads=[(tagp + "xn", b), "ident"], writes=[(tagp + "ps", pb)])
                P.op("dve", lambda e, b=b, k=k, pb=pb, hb=hb, sub=sub: e.tensor_scalar(
                    out=ht[hb][:, k, sub * 128:(sub + 1) * 128], in0=ps[pb][:, (k % 4) * 128:(k % 4 + 1) * 128],
                    scalar1=scl[:, k:k + 1], scalar2=modv[:, 0, k:k + 1], op0=ALU.mult, op1=ALU.add),
                    reads=[(tagp + "ps", pb), tagp + "scl", "modv"], writes=[(tagp + "ht", hb, sub)])
                if hook is not None:
                    hook("chunk", b, k, pb, ps, scl, r0)
            if hook is not None:
                hook("sub", b, None, None, ps, scl, r0)
        P.dma("sp", hT_d[:, :, t * 512:(t + 1) * 512], ht[hb][:], reads=[(tagp + "ht", hb, s_) for s_ in range(4)],
              writes=[("hT_d", t)])


def emit_proj(nc, P, st, S, hT_d, w_d, ncols, fm, z_d, tagp):
    wst = [st.enter_context(nc.sbuf_tensor("%s_wst%d" % (tagp, i), [128, 4, 512], F32)) for i in range(2)]
    wbf = [st.enter_context(nc.sbuf_tensor("%s_wbf%d" % (tagp, i), [128, 16, 512], BF16)) for i in range(2)]
    ht = [st.enter_context(nc.sbuf_tensor("%s_ht%d" % (tagp, i), [128, 16, 512], BF16)) for i in range(2)]
    ost = [st.enter_context(nc.sbuf_tensor("%s_ost%d" % (tagp, i), [128, 512], F32)) for i in range(4)]
    ps = [st.enter_context(nc.psum_tensor("%s_ps%d" % (tagp, i), [128, 512], F32)) for i in range(4)]
    ngrp = (ncols + 511) // 512
    hi = 0
    oi = 0
    wi = 0
    for g in range(ngrp):
        c0 = g * 512
        cw = min(512, ncols - c0)
        wb = g % 2
        for kq in range(4):
            sb = wi % 2
            wi += 1
            P.dma("act", wst[sb][:, :, 0:cw], w_d[:, kq * 4:(kq + 1) * 4, c0:c0 + cw], writes=[(tagp + "wst", sb)])
            P.op("pool", lambda e, sb=sb, wb=wb, kq=kq, cw=cw: e.tensor_copy(out=wbf[wb][:, kq * 4:(kq + 1) * 4, 0:cw], in_=wst[sb][:, :, 0:cw]),
                 reads=[(tagp + "wst", sb)], writes=[(tagp + "wbf", wb)])
        for t in range(S // 512):
            hb = hi % 2
            hi += 1
            P.dma("sp", ht[hb][:], hT_d[:, :, t * 512:(t + 1) * 512], reads=[("hT_d", t)], writes=[(tagp + "ht", hb)])
            nsub = (cw + 127) // 128 if fm else 4
            for j in range(nsub):
                ob = oi % 4
                oi += 1
                if fm:
                    m = min(128, cw - j * 128)
                    for k in range(16):
                        P.op("pe", lambda e, ob=ob, k=k, j=j, m=m, wb=wb, hb=hb: e.matmul(
                            ps[ob][0:m, :], lhsT=wbf[wb][:, k, j * 128:j * 128 + m], rhs=ht[hb][:, k, :], start=(k == 0), stop=(k == 15)),
                            reads=[(tagp + "wbf", wb), (tagp + "ht", hb)], writes=[(tagp + "ps", ob)])
                    eng = "act" if oi % 2 else "dve"
                    if eng == "act":
                        P.op("act", lambda e, ob=ob, m=m: e.copy(out=ost[ob][0:m, :], in_=ps[ob][0:m, :]),
                             reads=[(tagp + "ps", ob)], writes=[(tagp + "ost", ob)])
                    else:
                        P.op("dve", lambda e, ob=ob, m=m: e.tensor_copy(out=ost[ob][0:m, :], in_=ps[ob][0:m, :]),
                             reads=[(tagp + "ps", ob)], writes=[(tagp + "ost", ob)])
                    P.dma("sp", z_d[c0 + j * 128:c0 + j * 128 + m, t * 512:(t + 1) * 512], ost[ob][0:m, :],
                          reads=[(tagp + "ost", ob)], writes=[(tagp + "z", g, t)])
                else:
                    for k in range(16):
                        P.op("pe", lambda e, ob=ob, k=k, j=j, cw=cw, wb=wb, hb=hb: e.matmul(
                            ps[ob][:, 0:cw], lhsT=ht[hb][:, k, j * 128:(j + 1) * 128], rhs=wbf[wb][:, k, 0:cw], start=(k == 0), stop=(k == 15)),
                            reads=[(tagp + "wbf", wb), (tagp + "ht", hb)], writes=[(tagp + "ps", ob)])
                    eng = "act" if oi % 2 else "dve"
                    if eng == "act":
                        P.op("act", lambda e, ob=ob, cw=cw: e.copy(out=ost[ob][:, 0:cw], in_=ps[ob][:, 0:cw]),
                             reads=[(tagp + "ps", ob)], writes=[(tagp + "ost", ob)])
                    else:
                        P.op("dve", lambda e, ob=ob, cw=cw: e.tensor_copy(out=ost[ob][:, 0:cw], in_=ps[ob][:, 0:cw]),
                             reads=[(tagp + "ps", ob)], writes=[(tagp + "ost", ob)])
                    r0 = t * 512 + j * 128
                    P.dma("sp", z_d[r0:r0 + 128, c0:c0 + cw], ost[ob][:, 0:cw],
                          reads=[(tagp + "ost", ob)], writes=[(tagp + "z", g, t)])


def _scan_free(P, eng, bufs, keys, n, op):
    cur = 0
    d = 1
    while d < n:
        a, b = bufs[cur], bufs[1 - cur]
        ka, kb = keys[cur], keys[1 - cur]
        P.op(eng, lambda e, a=a, b=b, d=d: e.tensor_tensor(out=b[:, :, d:n], in0=a[:, :, d:n], in1=a[:, :, 0:n - d], op=op),
             reads=[ka], writes=[kb])
        P.op(eng, lambda e, a=a, b=b, d=d: e.tensor_copy(out=b[:, :, 0:d], in_=a[:, :, 0:d]), reads=[ka], writes=[kb])
        cur = 1 - cur
        d *= 2
    return cur


def emit_mlstm(nc, P, st, S, zfm_d, ztm_d, ya_d, cw_d, cb_d, gb_d, mg_d, tri, ones, ident, identb, epst):
    NC = S // 128
    W = 2 * NC
    sb = lambda name, shape, dt=F32: st.enter_context(nc.sbuf_tensor(name, shape, dt))
    G = sb("m_G", [128, NC, 4])
    gb = sb("m_gb", [128, 4])
    ngb = sb("m_ngb", [128, 4])
    LI = sb("m_LI", [128, 2, NC])
    LF = sb("m_LF", [128, 2, NC])
    Fg = sb("m_F", [128, 2, NC])
    sc0 = sb("m_sc0", [128, 2, NC])
    sc1 = sb("m_sc1", [128, 2, NC])
    U = sb("m_U", [128, 2, NC])
    Rb = sb("m_Rb", [128, 2, NC])
    Wt = sb("m_W", [128, 2, NC])
    CL = sb("m_CL", [128, 2, NC])
    DC = sb("m_DC", [128, 2, NC])
    cm = sb("m_cm", [128, 1])
    r0 = sb("m_r0", [1, 2, NC])
    r1 = sb("m_r1", [1, 2, NC])
    cw = sb("m_cw", [128, 4, 4])
    cb = sb("m_cb", [128, 4])
    mg = sb("m_mg", [128, 512])
    qT = [sb("m_qT%d" % h, [128, S], BF16) for h in range(2)]
    kT = [sb("m_kT%d" % h, [128, S], BF16) for h in range(2)]
    PL = min(S, 2048)
    xin = [sb("m_xin%d" % i, [128, 3 + PL]) for i in range(2)]
    acc = [sb("m_acc%d" % i, [128, PL]) for i in range(2)]
    ps = [st.enter_context(nc.psum_tensor("m_ps%d" % i, [128, 512], F32)) for i in range(7)]

    P.dma("sp", G[:], ztm_d[:, TM_IG:TM_IG + 4].rearrange("(c p) j -> p c j", p=128), reads=[("z", "tm")], writes=["G"])
    P.dma("sp", gb[:], gb_d, writes=["gb"])
    P.dma("sp", cw[:], cw_d, writes=["cw"])
    P.dma("sp", cb[:], cb_d, writes=["cb"])
    P.dma("sp", mg[:], mg_d, writes=["mg"])
    P.op("dve", lambda e: e.tensor_scalar(out=ngb[:], in0=gb[:], scalar1=-1.0, scalar2=None, op0=ALU.mult), reads=["gb"], writes=["ngb"])
    for h in range(2):
        P.op("dve", lambda e, h=h: e.tensor_scalar(out=LI[:, h, :], in0=G[:, :, h], scalar1=gb[:, h:h + 1], scalar2=None, op0=ALU.add),
             reads=["G", "gb"], writes=["LI"])
        P.op("act", lambda e, h=h: e.activation(out=sc0[:, h, :], in_=G[:, :, 2 + h], func=AF.Exp, scale=-1.0, bias=ngb[:, 2 + h:3 + h]),
             reads=["G", "ngb"], writes=["sc0"])
    P.op("act", lambda e: e.activation(out=sc1[:], in_=sc0[:], func=AF.Ln, bias=1.0), reads=["sc0"], writes=["sc1"])
    P.op("dve", lambda e: e.tensor_scalar(out=LF[:], in0=sc1[:], scalar1=-1.0, scalar2=None, op0=ALU.mult), reads=["sc1"], writes=["LF"])
    LFf = LF[:].rearrange("p h c -> p (h c)")
    P.op("pe", lambda e: e.matmul(ps[0][:, 0:W], lhsT=tri[:], rhs=LFf, start=True, stop=True), reads=["LF", "tri"], writes=[("mps", 0)])
    P.op("pe", lambda e: e.matmul(ps[1][:, 0:W], lhsT=ones[:], rhs=LFf, start=True, stop=True), reads=["LF", "ones"], writes=[("mps", 1)])
    P.op("dve", lambda e: e.tensor_copy(out=sc0[:].rearrange("p h c -> p (h c)"), in_=ps[1][:, 0:W]), reads=[("mps", 1)], writes=["sc0"])
    P.op("dve", lambda e: e.tensor_tensor(out=Fg[:].rearrange("p h c -> p (h c)"), in0=ps[0][:, 0:W], in1=sc0[:].rearrange("p h c -> p (h c)"),
                                         op=ALU.subtract), reads=[("mps", 0), "sc0"], writes=["F"])
    r = _scan_free(P, "dve", [sc0, sc1], ["sc0", "sc1"], NC, ALU.add)
    incl = [sc0, sc1][r]
    P.op("dve", lambda e: e.tensor_tensor(out=Fg[:], in0=Fg[:], in1=incl[:], op=ALU.add), reads=["F", ["sc0", "sc1"][r]], writes=["F"])
    P.op("dve", lambda e: e.tensor_tensor(out=U[:], in0=LI[:], in1=Fg[:], op=ALU.subtract), reads=["LI", "F"], writes=["U"])
    P.op("pe", lambda e: e.transpose(out=ps[2][0:W, 0:128], in_=U[:].rearrange("p h c -> p (h c)"), identity=ident[:]),
         reads=["U", "ident"], writes=[("mps", 2)])
    P.op("dve", lambda e: e.tensor_reduce(out=cm[0:W, :], in_=ps[2][0:W, 0:128], axis=AX.X, op=ALU.max), reads=[("mps", 2)], writes=["cm"])
    P.op("pe", lambda e: e.transpose(out=ps[3][0:1, 0:W], in_=cm[0:W, :], identity=ident[0:W, 0:W]), reads=["cm", "ident"], writes=[("mps", 3)])
    P.op("dve", lambda e: e.tensor_copy(out=r0[:].rearrange("p h c -> p (h c)"), in_=ps[3][0:1, 0:W]), reads=[("mps", 3)], writes=["r0"])
    r = _scan_free(P, "dve", [r0, r1], ["r0", "r1"], NC, ALU.max)
    rr = [r0, r1][r]
    P.op("pe", lambda e: e.matmul(ps[4][:, 0:W], lhsT=ones[0:1, :], rhs=rr[:].rearrange("p h c -> p (h c)"), start=True, stop=True),
         reads=[["r0", "r1"][r], "ones"], writes=[("mps", 4)])
    P.op("dve", lambda e: e.tensor_copy(out=Rb[:].rearrange("p h c -> p (h c)"), in_=ps[4][:, 0:W]), reads=[("mps", 4)], writes=["Rb"])
    P.op("dve", lambda e: e.tensor_tensor(out=sc0[:], in0=U[:], in1=Rb[:], op=ALU.subtract), reads=["U", "Rb"], writes=["sc0"])
    P.op("act", lambda e: e.activation(out=Wt[:], in_=sc0[:], func=AF.Exp), reads=["sc0"], writes=["Wt"])
    P.op("dve", lambda e: e.tensor_tensor(out=sc1[:], in0=Fg[:], in1=Rb[:], op=ALU.add), reads=["F", "Rb"], writes=["sc1"])
    P.op("act", lambda e: e.activation(out=CL[:], in_=sc1[:], func=AF.Exp, scale=-1.0), reads=["sc1"], writes=["CL"])
    P.op("dve", lambda e: e.memset(DC[:], 0.0), writes=["DC"])
    if NC > 1:
        P.op("dve", lambda e: e.tensor_tensor(out=DC[:, :, 1:NC], in0=Rb[:, :, 0:NC - 1], in1=Rb[:, :, 1:NC], op=ALU.subtract),
             reads=["Rb"], writes=["DC"])
    P.op("act", lambda e: e.activation(out=DC[:], in_=DC[:], func=AF.Exp), reads=["DC"], writes=["DC"])

    it = 0
    for qk in range(2):
        for h in range(2):
            ch = qk * 2 + h
            row0 = (FM_MQ if qk == 0 else FM_MK) + h * 128
            dst = qT[h] if qk == 0 else kT[h]
            for pc in range(S // PL):
                b = it % 2
                it += 1
                p0 = pc * PL
                if pc == 0:
                    P.op("pool", lambda e, b=b: e.memset(xin[b][:, 0:3], 0.0), writes=[("xin", b)])
                    P.dma("sp", xin[b][:, 3:3 + PL], zfm_d[row0:row0 + 128, 0:PL], reads=[("z", "fm")], writes=[("xin", b)])
                else:
                    P.dma("sp", xin[b][:], zfm_d[row0:row0 + 128, p0 - 3:p0 + PL], reads=[("z", "fm")], writes=[("xin", b)])
                P.op("dve", lambda e, b=b, ch=ch: e.tensor_scalar(out=acc[b][:], in0=xin[b][:, 0:PL], scalar1=cw[:, ch, 0:1], scalar2=cb[:, ch:ch + 1],
                                                               op0=ALU.mult, op1=ALU.add), reads=[("xin", b), "cw", "cb"], writes=[("acc", b)])
                for j in range(1, 4):
                    P.op("dve", lambda e, b=b, ch=ch, j=j: e.scalar_tensor_tensor(out=acc[b][:], in0=xin[b][:, j:j + PL], scalar=cw[:, ch, j:j + 1],
                                                                                in1=acc[b][:], op0=ALU.mult, op1=ALU.add),
                         reads=[("xin", b), "cw", ("acc", b)], writes=[("acc", b)])
                if qk == 0:
                    P.op("act", lambda e, b=b: e.activation(out=acc[b][:], in_=acc[b][:], func=AF.Silu), reads=[("acc", b)], writes=[("acc", b)])
                    P.op("dve", lambda e, b=b, dst=dst, p0=p0: e.tensor_scalar(out=dst[:, p0:p0 + PL], in0=acc[b][:], scalar1=128.0 ** -0.5, scalar2=None,
                                                                             op0=ALU.mult), reads=[("acc", b)], writes=[("qk", qk, h)])
                else:
                    P.op("act", lambda e, b=b, dst=dst, p0=p0: e.activation(out=dst[:, p0:p0 + PL], in_=acc[b][:], func=AF.Silu),
                         reads=[("acc", b)], writes=[("qk", qk, h)])

    S32 = [sb("m_S32_%d" % h, [128, 257]) for h in range(2)]
    T32 = [sb("m_T32_%d" % h, [128, 257]) for h in range(2)]
    Tbf = [sb("m_Tbf_%d" % h, [128, 257], BF16) for h in range(2)]
    vin = [sb("m_vin%d" % i, [128, 512]) for i in range(2)]
    oin = [sb("m_oin%d" % i, [128, 512]) for i in range(2)]
    vwt = [sb("m_vw%d" % h, [128, 257], BF16) for h in range(2)]
    ktk = [sb("m_ktk%d" % h, [128, 128], BF16) for h in range(2)]
    AT = [sb("m_AT%d" % h, [128, 128], BF16) for h in range(2)]
    hm = [sb("m_hm%d" % h, [128, 256]) for h in range(2)]
    hq = [sb("m_hq%d" % h, [128, 256]) for h in range(2)]
    sg = [sb("m_sg%d" % h, [128, 256]) for h in range(2)]
    yo = [sb("m_yo%d" % i, [128, 512]) for i in range(2)]
    stt = [sb("m_stt%d" % h, [128, 4]) for h in range(2)]
    for h in range(2):
        P.op("dve", lambda e, h=h: e.memset(S32[h][:], 0.0), writes=[("S32", h)])
    psb = st.enter_context(nc.psum_tensor("m_psb", [128, 2, 128], BF16))
    for c in range(NC):
        ib = c % 2
        t0 = c * 128
        P.dma("sp", vin[ib][:], ztm_d[t0:t0 + 128, TM_MV:TM_MV + 512], reads=[("z", "tm")], writes=[("vin", ib)])
        P.dma("sp", oin[ib][:], ztm_d[t0:t0 + 128, TM_OG:TM_OG + 512], reads=[("z", "tm")], writes=[("oin", ib)])
        for h in range(2):
            P.op("dve", lambda e, h=h, ib=ib, c=c: e.tensor_scalar(out=vwt[h][:, 0:256], in0=vin[ib][:, h * 256:(h + 1) * 256], scalar1=Wt[:, h, c:c + 1],
                                                                  scalar2=None, op0=ALU.mult), reads=[("vin", ib), "Wt"], writes=[("vw", h)])
            P.op("dve", lambda e, h=h, c=c: e.tensor_copy(out=vwt[h][:, 256:257], in_=Wt[:, h, c:c + 1]), reads=["Wt"], writes=[("vw", h)])
            P.op("pe", lambda e, h=h, t0=t0: e.transpose(out=psb[:, h, :], in_=kT[h][:, t0:t0 + 128], identity=identb[:]),
                 reads=[("qk", 1, h), "identb"], writes=[("psb", h)])
            P.op("act", lambda e, h=h: e.copy(out=ktk[h][:], in_=psb[:, h, :]), reads=[("psb", h)], writes=[("ktk", h)])
            P.op("pe", lambda e, h=h, t0=t0: e.matmul(ps[h][:, 0:128], lhsT=kT[h][:, t0:t0 + 128], rhs=qT[h][:, t0:t0 + 128], start=True, stop=True),
                 reads=[("qk", 1, h), ("qk", 0, h)], writes=[("cps", h)])
            P.op("dve", lambda e, h=h: e.tensor_tensor(out=AT[h][:], in0=ps[h][:, 0:128], in1=tri[:], op=ALU.mult),
                 reads=[("cps", h), "tri"], writes=[("AT", h)])
            P.op("dve", lambda e, h=h, c=c: e.tensor_scalar(out=T32[h][:], in0=S32[h][:], scalar1=DC[:, h, c:c + 1], scalar2=None, op0=ALU.mult),
                 reads=[("S32", h), "DC"], writes=[("T32", h)])
            P.op("act", lambda e, h=h: e.copy(out=Tbf[h][:], in_=T32[h][:]), reads=[("T32", h)], writes=[("Tbf", h)])
            P.op("pe", lambda e, h=h: e.matmul(ps[2 + h][:, 0:257], lhsT=AT[h][:], rhs=vwt[h][:], start=True, stop=False),
                 reads=[("AT", h), ("vw", h)], writes=[("nps", h)])
            P.op("pe", lambda e, h=h, t0=t0: e.matmul(ps[2 + h][:, 0:257], lhsT=qT[h][:, t0:t0 + 128], rhs=Tbf[h][:], start=False, stop=True),
                 reads=[("qk", 0, h), ("Tbf", h)], writes=[("nps", h)])
            P.op("pe", lambda e, h=h: e.matmul(ps[4 + h][:, 0:257], lhsT=ktk[h][:], rhs=vwt[h][:], start=True, stop=True),
                 reads=[("ktk", h), ("vw", h)], writes=[("sps", h)])
            P.op("dve", lambda e, h=h: e.tensor_tensor(out=S32[h][:], in0=ps[4 + h][:, 0:257], in1=T32[h][:], op=ALU.add),
                 reads=[("sps", h), ("T32", h)], writes=[("S32", h)])
            P.op("act", lambda e, h=h: e.activation(out=stt[h][:, 0:1], in_=ps[2 + h][:, 256:257], func=AF.Abs),
                 reads=[("nps", h)], writes=[("stt", h, 0)])
            P.op("dve", lambda e, h=h, c=c: e.tensor_scalar(out=stt[h][:, 0:1], in0=stt[h][:, 0:1], scalar1=CL[:, h, c:c + 1], scalar2=None,
                                                          op0=ALU.max), reads=[("stt", h, 0), "CL"], writes=[("stt", h, 0)])
            P.op("dve", lambda e, h=h: e.reciprocal(out=stt[h][:, 1:2], in_=stt[h][:, 0:1]), reads=[("stt", h, 0)], writes=[("stt", h, 1)])
            P.op("dve", lambda e, h=h: e.tensor_scalar(out=hm[h][:], in0=ps[2 + h][:, 0:256], scalar1=stt[h][:, 1:2], scalar2=None, op0=ALU.mult),
                 reads=[("nps", h), ("stt", h, 1)], writes=[("hm", h)])
            P.op("act", lambda e, h=h: e.activation(out=hq[h][:], in_=hm[h][:], func=AF.Square, scale=1.0 / 16.0, accum_out=stt[h][:, 2:3]),
                 reads=[("hm", h)], writes=[("hq", h), ("stt", h, 2)])
            P.op("act", lambda e, h=h: e.activation(out=stt[h][:, 2:3], in_=stt[h][:, 2:3], func=AF.Sqrt, bias=epst[:, 0:1]),
                 reads=[("stt", h, 2), "epst"], writes=[("stt", h, 2)])
            P.op("dve", lambda e, h=h: e.reciprocal(out=stt[h][:, 3:4], in_=stt[h][:, 2:3]),
                 reads=[("stt", h, 2)], writes=[("stt", h, 3)])
            P.op("dve", lambda e, h=h: e.scalar_tensor_tensor(out=hq[h][:], in0=hm[h][:], scalar=stt[h][:, 3:4], in1=mg[:, h * 256:(h + 1) * 256],
                                                             op0=ALU.mult, op1=ALU.mult), reads=[("hm", h), ("stt", h, 3), "mg"], writes=[("hq", h)])
            P.op("act", lambda e, h=h, ib=ib: e.activation(out=sg[h][:], in_=oin[ib][:, h * 256:(h + 1) * 256], func=AF.Sigmoid),
                 reads=[("oin", ib)], writes=[("sg", h)])
            P.op("pool", lambda e, h=h, ib=ib: e.tensor_tensor(out=yo[ib][:, h * 256:(h + 1) * 256], in0=hq[h][:], in1=sg[h][:], op=ALU.mult),
                 reads=[("hq", h), ("sg", h)], writes=[("yo", ib, h)])
        P.dma("sp", ya_d[t0:t0 + 128, :], yo[ib][:], reads=[("yo", ib, 0), ("yo", ib, 1)], writes=[("ya", c)])


def build_la(S, debug=False, with_nsa=True, nph=99):
    nc = _new_nc()
    din = lambda name, shape, dt=F32: nc.dram_tensor(name, shape, dt, kind="ExternalInput").ap()
    x = din("x", [S, D])
    modv_d = din("modv", [128, 2, 16])
    g_d = din("g1", [128, 16])
    ident_d = din("ident", [128, 128])
    tri_d = din("tri", [128, 128])
    wfm_d = din("wfm", [128, 16, NFM])
    wtm_d = din("wtm", [128, 16, NTM])
    cw_d = din("convw", [128, 4, 4])
    cb_d = din("convb", [128, 4])
    gb_d = din("gateb", [128, 4])
    mg_d = din("mnormg", [128, 512])
    NS_ = S // 64
    NCC_ = ((S - 32) // 16 + 1 + 127) // 128
    nd = {}
    if with_nsa:
        nd = dict(bc=din("bc", [NBC, 128, 512]), bs=din("bs", [10, 128, 512]), wsel=din("wsel", [128, 256]), ew=din("ew", [128, S]),
                  ov=din("ov", [128, NCC_, NS_]), qg=din("qg", [128, 1]), kg=din("kg", [128, 1]), posT=din("posT", [128, 32]),
                  ckw1=din("ckw1", [128, 32, 128]), ckw2=din("ckw2", [128, 128]), cvw1=din("cvw1", [128, 32, 128]), cvw2=din("cvw2", [128, 128]))
        yb_d = nc.dram_tensor("yb", [S, 512], F32, kind="ExternalOutput").ap()
    kind = "ExternalOutput" if debug else "Internal"
    hT_d = nc.dram_tensor("hT_d", [128, 16, S], BF16, kind=kind).ap()
    zfm_d = nc.dram_tensor("zfm_d", [NFM, S], F32, kind=kind).ap()
    ztm_d = nc.dram_tensor("ztm_d", [S, NTM], F32, kind=kind).ap()
    ya_d = nc.dram_tensor("ya", [S, 512], F32, kind="ExternalOutput").ap()
    with contextlib.ExitStack() as st0:
        P = Prog(nc, st0)
        ident = st0.enter_context(nc.sbuf_tensor("ident_s", [128, 128], F32))
        identb = st0.enter_context(nc.sbuf_tensor("identb_s", [128, 128], BF16))
        tri = st0.enter_context(nc.sbuf_tensor("tri_s", [128, 128], F32))
        ones = st0.enter_context(nc.sbuf_tensor("ones_s", [128, 128], F32))
        modv = st0.enter_context(nc.sbuf_tensor("modv_s", [128, 2, 16], F32))
        gv = st0.enter_context(nc.sbuf_tensor("gv_s", [128, 16], F32))
        epst = st0.enter_context(nc.sbuf_tensor("eps_s", [128, 1], F32))
        kcmpT = st0.enter_context(nc.sbuf_tensor("kcmpT_s", [128, NCC_ * 128], BF16))
        vcmp = st0.enter_context(nc.sbuf_tensor("vcmp_s", [128, NCC_, 130], BF16))

        def consts():
            P.dma("sp", ident[:], ident_d, writes=["ident"])
            P.dma("sp", tri[:], tri_d, writes=["tri"])
            P.dma("sp", modv[:], modv_d, writes=["modv"])
            P.dma("sp", gv[:], g_d, writes=["gv"])
            P.op("dve", lambda e: e.memset(ones[:], 1.0), writes=["ones"])
            P.op("dve", lambda e: e.memset(epst[:], EPS), writes=["epst"])
            P.op("dve", lambda e: e.tensor_copy(out=identb[:], in_=ident[:]), reads=["ident"], writes=["identb"])

        def reconst():
            pass

        consts()
        with contextlib.ExitStack() as st:
            emit_norm_to_hT(nc, P, st, x, S, modv, gv, ident, epst, hT_d)
            P.flush()
        if nph >= 2:
          with contextlib.ExitStack() as st:
            emit_proj(nc, P, st, S, hT_d, wfm_d, NFM, True, zfm_d, "pf")
            P.flush()
        if nph >= 3:
          with contextlib.ExitStack() as st:
            emit_proj(nc, P, st, S, hT_d, wtm_d, NTM, False, ztm_d, "pt")
            P.flush()
        if nph >= 4:
          with contextlib.ExitStack() as st:
            emit_mlstm(nc, P, st, S, zfm_d, ztm_d, ya_d, cw_d, cb_d, gb_d, mg_d, tri, ones, ident, identb, epst)
            P.flush()
        if nph >= 5 and with_nsa:
          with contextlib.ExitStack() as st:
            emit_nsa_compress(nc, P, st, S, zfm_d, nd, ones, epst, kcmpT, vcmp)
            P.flush()
          with contextlib.ExitStack() as st:
            emit_nsa(nc, P, st, S, zfm_d, ztm_d, yb_d, nd, ones, ident, identb, epst, kcmpT, vcmp)
            P.flush()
    return nc


def la_inputs(l, b, hh, S, x_b, mod, inp):
    fm, tm = la_columns(hh)
    m = mod[l, b]
    sh1, sc1 = m[0:D], m[D:2 * D]
    pk = lambda v: np.ascontiguousarray(v.reshape(16, 128).T)
    w_in = inp["w_in"][l]
    wl = lambda cols: np.ascontiguousarray(w_in[:, cols].reshape(16, 128, len(cols)).transpose(1, 0, 2))
    wtm = wl(tm)
    wtm[:, :, NTM - 12:] = 0.0
    cwl = inp["conv_w"][l]
    cbl = inp["conv_b"][l]
    chs = [hh * 256, hh * 256 + 128, 512 + hh * 256, 512 + hh * 256 + 128]
    convw = np.stack([cwl[:, c0:c0 + 128].T for c0 in chs], axis=1)
    convb = np.stack([cbl[c0:c0 + 128] for c0 in chs], axis=1)
    gbv = inp["mlstm_gate_b"][l]
    gate = np.array([gbv[hh * 2], gbv[hh * 2 + 1], gbv[4 + hh * 2], gbv[4 + hh * 2 + 1]], np.float32)
    d = {
        "x": np.ascontiguousarray(x_b[:S]),
        "modv": np.ascontiguousarray(np.stack([pk(sh1), pk(sc1)], axis=1)),
        "g1": pk(inp["norm1_g"][l]),
        "ident": np.eye(128, dtype=np.float32),
        "tri": np.triu(np.ones((128, 128), np.float32)),
        "wfm": wl(fm), "wtm": wtm,
        "convw": np.ascontiguousarray(convw), "convb": np.ascontiguousarray(convb),
        "gateb": np.ascontiguousarray(np.broadcast_to(gate[None, :], (128, 4))),
        "mnormg": np.ascontiguousarray(np.broadcast_to(inp["mlstm_norm_g"][l][None, hh * 512:(hh + 1) * 512], (128, 512))),
    }
    if "rel_bias" in inp:
        bc, bs, wsel, ew, ov = nsa_tables(S, inp["rel_bias"][:, hh * 4:(hh + 1) * 4])
        col = lambda v: np.ascontiguousarray(v.reshape(128, 1))
        w1l = lambda w: np.ascontiguousarray(w.reshape(32, 128, 128).transpose(1, 0, 2))
        d.update(bc=bc, bs=bs, wsel=wsel, ew=ew, ov=ov, qg=col(inp["qnorm_g"][l]), kg=col(inp["knorm_g"][l]),
                 posT=np.ascontiguousarray(inp["cmp_pos"][l].T), ckw1=w1l(inp["cmp_k_w1"][l]), ckw2=np.ascontiguousarray(inp["cmp_k_w2"][l]),
                 cvw1=w1l(inp["cmp_v_w1"][l]), cvw2=np.ascontiguousarray(inp["cmp_v_w2"][l]))
    return d


BIG = 30000.0
NBC = 24


def t5_bucket_np(dist):
    dist = np.maximum(dist, 0)
    d1 = np.maximum(dist, 1).astype(np.float32)
    lr = np.log(d1 / np.float32(16.0)) / np.float32(np.log(64.0))
    large = np.minimum(16 + (lr * np.float32(16.0)).astype(np.int32), 31)
    return np.where(dist < 16, dist, large)


def nsa_tables(S, rel_bias_g):
    rb = rel_bias_g.astype(np.float32)
    neg = np.float32(-BIG)
    ti = np.arange(128)
    bc = np.empty((NBC, 128, 4, 128), np.float32)
    ci = np.arange(128)
    for dj in range(NBC):
        if dj == NBC - 1:
            bc[dj] = rb[31][None, :, None]
            continue
        dist = 128 * dj + ti[None, :] - 16 * ci[:, None] - 31
        val = rb[t5_bucket_np(dist)]
        val = np.where((dist >= 0)[:, :, None], val, neg)
        bc[dj] = val.transpose(0, 2, 1)
    bs = np.empty((10, 128, 4, 128), np.float32)
    ki = np.arange(128)
    for dl in range(10):
        if dl == 8:
            bs[dl] = rb[31][None, :, None]
            continue
        dd = 4 if dl == 9 else dl
        dist = 128 * dd + ti[None, :] - ki[:, None]
        val = rb[t5_bucket_np(dist)]
        ok = dist >= 0
        if dl == 9:
            ok = ok & (dist < 512)
        val = np.where(ok[:, :, None], val, neg)
        bs[dl] = val.transpose(0, 2, 1)
    m = np.arange(256)[None, :] - 128
    br = (ti[:, None] >= 64).astype(np.int64)
    wsel = np.where(m > br, np.float32(-1e30), np.where((m == br) | (m == br - 1), np.float32(1e4), np.float32(0.0))).astype(np.float32)
    NS = S // 64
    ew = (np.arange(128)[:, None] == (np.arange(S)[None, :] // 64)).astype(np.float32)
    NCMP = (S - 32) // 16 + 1
    NCC = (NCMP + 127) // 128
    cst = np.arange(NCC * 128) * 16
    sst = np.arange(NS) * 64
    ovl = np.clip(np.minimum(cst[:, None] + 32, sst[None, :] + 64) - np.maximum(cst[:, None], sst[None, :]), 0, None).astype(np.float32) / 32.0
    ovl[NCMP:] = 0.0
    ov = np.ascontiguousarray(ovl.reshape(NCC, 128, NS).transpose(1, 0, 2))
    return (np.ascontiguousarray(bc.reshape(NBC, 128, 512)), np.ascontiguousarray(bs.reshape(10, 128, 512)),
            np.ascontiguousarray(wsel), np.ascontiguousarray(ew), ov)


def emit_nsa(nc, P, st, S, zfm_d, ztm_d, yb_d, din, ones, ident, identb, epst, kcmpT, vcmp):
    NQB = S // 128
    NS = S // 64
    NCMP = (S - 32) // 16 + 1
    NCC = (NCMP + 127) // 128
    NSEL = min(16, NS)
    sb = lambda name, shape, dt=F32: st.enter_context(nc.sbuf_tensor(name, shape, dt))
    stg = [sb("a_stg%d" % i, [128, 1024]) for i in range(2)]
    Bc = sb("a_Bc", [128, NBC, 512], BF16)
    Bs = sb("a_Bs", [128, 10, 512], BF16)
    Ew = sb("a_Ew", [128, S], BF16)
    ovb = sb("a_ov", [128, NCC, NS], BF16)
    wsel = sb("a_wsel", [128, 256])
    qg = sb("a_qg", [128, 1])
    kg = sb("a_kg", [128, 1])
    ksn = sb("a_ksn", [128, S], BF16)
    kwn = sb("a_kwn", [128, S], BF16)
    vsb = sb("a_vsb", [128, NQB, 130], BF16)
    vwb = sb("a_vwb", [128, NQB, 130], BF16)
    ps = [st.enter_context(nc.psum_tensor("a_ps%d" % i, [128, 512], F32)) for i in range(8)]
    SB0, SB1, OA0, OA1, OB0, OB1, UB, MB = range(8)
    cnt = {"stg": 0}

    def load_cast(dst, src, n, eng="pool"):
        b = cnt["stg"] % 2
        cnt["stg"] += 1
        P.dma("sp", stg[b][:, 0:n], src, writes=[("stg", b)])
        P.op(eng, lambda e: e.tensor_copy(out=dst, in_=stg[b][:, 0:n]), reads=[("stg", b)], writes=["cst"])

    for i in range(NBC):
        load_cast(Bc[:, i, :], din["bc"][i], 512)
    for i in range(10):
        load_cast(Bs[:, i, :], din["bs"][i], 512)
    for i in range(S // 1024 if S >= 1024 else 1):
        n = min(1024, S)
        load_cast(Ew[:, i * n:(i + 1) * n], din["ew"][:, i * n:(i + 1) * n], n)
    for cc in range(NCC):
        load_cast(ovb[:, cc, :], din["ov"][:, cc, :], NS)
    P.dma("sp", wsel[:], din["wsel"], writes=["cst"])
    P.dma("sp", qg[:], din["qg"], writes=["qg"])
    P.dma("sp", kg[:], din["kg"], writes=["cst"])
    P.op("dve", lambda e: e.tensor_scalar(out=qg[:], in0=qg[:], scalar1=128.0 ** -0.5, scalar2=None, op0=ALU.mult), reads=["qg"], writes=["cst"])

    sq = [sb("a_sq%d" % i, [128, 512]) for i in range(2)]
    rs = [sb("a_rs%d" % i, [128, 512]) for i in range(2)]
    ncnt = {"n": 0}

    def norm_fm(xap, gap, dst, rkeys, wkeys):
        b = ncnt["n"] % 2
        ncnt["n"] += 1
        P.op("act", lambda e: e.activation(out=sq[b][:], in_=xap, func=AF.Square), reads=rkeys, writes=[("sq", b)])
        P.op("pe", lambda e: e.matmul(ps[MB][:], lhsT=ones[:], rhs=sq[b][:], start=True, stop=True), reads=[("sq", b), "cst0"], writes=[("ps", MB)])
        P.op("act", lambda e: e.activation(out=rs[b][:], in_=ps[MB][:], func=AF.Sqrt, scale=1.0 / 128.0, bias=epst[:, 0:1]),
             reads=[("ps", MB)], writes=[("rs", b)])
        P.op("dve", lambda e: e.reciprocal(out=rs[b][:], in_=rs[b][:]), reads=[("rs", b)], writes=[("rs", b)])
        P.op("dve", lambda e: e.scalar_tensor_tensor(out=dst, in0=xap, scalar=gap, in1=rs[b][:], op0=ALU.mult, op1=ALU.mult),
             reads=list(rkeys) + [("rs", b), "cst"], writes=wkeys)

    xk = [sb("a_xk%d" % i, [128, 512]) for i in range(2)]
    it = 0
    for (row0, dst) in ((FM_KS, ksn), (FM_KW, kwn)):
        for t in range(S // 512):
            b = it % 2
            it += 1
            P.dma("sp", xk[b][:], zfm_d[row0:row0 + 128, t * 512:(t + 1) * 512], writes=[("xk", b)])
            norm_fm(xk[b][:], kg[:, 0:1], dst[:, t * 512:(t + 1) * 512], [("xk", b)], ["ksn"])
    P.op("pool", lambda e: e.memset(vsb[:, :, 128:130], 1.0), writes=["vsb"])
    P.op("pool", lambda e: e.memset(vwb[:, :, 128:130], 1.0), writes=["vsb"])
    CH = 4
    for c0 in range(0, NQB, CH):
        b = cnt["stg"] % 2
        cnt["stg"] += 1
        n = min(CH, NQB - c0)
        sv = stg[b][:, 0:n * 256].rearrange("p (c j) -> p c j", j=256)
        P.dma("sp", sv, ztm_d[c0 * 128:(c0 + n) * 128, TM_VS:TM_VS + 256].rearrange("(c p) j -> p c j", p=128), writes=[("stg", b)])
        P.op("pool", lambda e, sv=sv, c0=c0, n=n: e.tensor_copy(out=vsb[:, c0:c0 + n, 0:128], in_=sv[:, :, 0:128]), reads=[("stg", b)], writes=["vsb"])
        P.op("dve", lambda e, sv=sv, c0=c0, n=n: e.tensor_copy(out=vwb[:, c0:c0 + n, 0:128], in_=sv[:, :, 128:256]), reads=[("stg", b)], writes=["vsb"])

    qst = [sb("a_qst%d" % i, [128, 4, 512]) for i in range(1)]
    qnb = [sb("a_qnb%d" % i, [128, 4, 512], BF16) for i in range(2)]
    gt = [sb("a_gt%d" % i, [128, 12]) for i in range(2)]
    gsg = [sb("a_gsg%d" % i, [128, 12]) for i in range(2)]
    pT = [sb("a_pT%d" % i, [128, 512], BF16) for i in range(3)]
    score = sb("a_score", [128, 128])
    sc2 = sb("a_sc2", [128, 128])
    m8a = sb("a_m8a", [128, 8])
    m8b = sb("a_m8b", [128, 8])
    Msel = sb("a_M", [128, 128])
    pen4 = sb("a_pen4", [128, 4, 128], BF16)
    den4 = sb("a_den4", [128, 4])
    rc4 = sb("a_rc4", [128, 4])
    sc4 = sb("a_sc4", [128, 4])
    acc = [sb("a_acc%d" % i, [128, 512]) for i in range(2)]
    pcnt = {"p": 0, "s": 0}

    def attn_tile(mm_list, vtile, obanks, first, last, extra=None):
        sbk = (SB0, SB1)[pcnt["s"] % 2]
        pcnt["s"] += 1
        pb = pcnt["p"] % 3
        pcnt["p"] += 1
        for i, (l_, r_, rk) in enumerate(mm_list):
            P.op("pe", lambda e, l_=l_, r_=r_, i=i: e.matmul(ps[sbk][:], lhsT=l_, rhs=r_, start=(i == 0), stop=(i == len(mm_list) - 1)),
                 reads=rk, writes=[("ps", sbk)])
        P.op("act", lambda e: e.activation(out=pT[pb][:], in_=ps[sbk][:], func=AF.Exp), reads=[("ps", sbk)], writes=[("pT", pb)])
        for h in range(4):
            ob = obanks[h // 2]
            P.op("pe", lambda e, h=h, ob=ob: e.matmul(ps[ob][:, (h % 2) * 256:(h % 2) * 256 + 129], lhsT=pT[pb][:, h * 128:(h + 1) * 128], rhs=vtile,
                                                    start=(first and h % 2 == 0), stop=(last and h % 2 == 1)),
                 reads=[("pT", pb), "vsb", "cmp"], writes=[("ps", ob)])
        if extra is not None:
            extra(pb)

    def finish_branch(obanks, br, gs, ab, first_branch):
        for hb in range(2):
            P.op("dve", lambda e, hb=hb: e.tensor_copy(out=den4[:, hb * 2:hb * 2 + 2], in_=ps[obanks[hb]][:, 128:512:256]),
                 reads=[("ps", obanks[hb])], writes=["den4"])
        P.op("dve", lambda e: e.tensor_scalar(out=den4[:], in0=den4[:], scalar1=1e-30, scalar2=None, op0=ALU.max), reads=["den4"], writes=["den4"])
        P.op("dve", lambda e: e.reciprocal(out=sc4[:], in_=den4[:]), reads=["den4"], writes=["sc4"])
        if br == 0:
            P.op("dve", lambda e: e.tensor_copy(out=rc4[:], in_=sc4[:]), reads=["sc4"], writes=["rc4"])
        P.op("dve", lambda e: e.tensor_tensor(out=sc4[:], in0=sc4[:], in1=gs[:, br:12:3], op=ALU.mult), reads=["sc4", "gsg"], writes=["sc4"])
        for h in range(4):
            ob = obanks[h // 2]
            src = ps[ob][:, (h % 2) * 256:(h % 2) * 256 + 128]
            if first_branch:
                P.op("dve", lambda e, h=h, src=src: e.tensor_scalar(out=acc[ab][:, h * 128:(h + 1) * 128], in0=src, scalar1=sc4[:, h:h + 1], scalar2=None,
                                                                   op0=ALU.mult), reads=[("ps", ob), "sc4"], writes=[("acc", ab, h)])
            else:
                P.op("dve", lambda e, h=h, src=src: e.scalar_tensor_tensor(out=acc[ab][:, h * 128:(h + 1) * 128], in0=src, scalar=sc4[:, h:h + 1],
                                                                         in1=acc[ab][:, h * 128:(h + 1) * 128], op0=ALU.mult, op1=ALU.add),
                     reads=[("ps", ob), "sc4", ("acc", ab, h)], writes=[("acc", ab, h)])

    for qgi in range(S // 512):
        qb = qgi % 2
        t0 = qgi * 512
        for h in range(4):
            P.dma("sp", qst[0][:, h, :], zfm_d[FM_NQ + h * 128:FM_NQ + (h + 1) * 128, t0:t0 + 512], writes=[("qst", h)])
            norm_fm(qst[0][:, h, :], qg[:, 0:1], qnb[qb][:, h, :], [("qst", h)], [("qnb", qb)])
        for jj in range(4):
            j = qgi * 4 + jj
            ab = j % 2
            gb_ = j % 2
            r0 = j * 128
            P.dma("sp", gt[gb_][:], ztm_d[r0:r0 + 128, TM_GN:TM_GN + 12], writes=[("gt", gb_)])
            P.op("act", lambda e, gb_=gb_: e.activation(out=gsg[gb_][:], in_=gt[gb_][:], func=AF.Sigmoid), reads=[("gt", gb_)], writes=["gsg"])
            gs = gsg[gb_]
            rq = qnb[qb][:, :, jj * 128:(jj + 1) * 128]
            ncv = min(8 * j + 7, NCMP)
            ncc = (ncv + 127) // 128
            for cc in range(ncc):
                dj = j - 16 * cc
                bi = dj if dj < NBC - 1 else NBC - 1

                def extra(pb, cc=cc, ncc=ncc):
                    for h in range(4):
                        P.op("pe", lambda e, h=h: e.matmul(ps[UB][:, h * 128:h * 128 + NS], lhsT=pT[pb][:, h * 128:(h + 1) * 128], rhs=ovb[:, cc, :],
                                                          start=(cc == 0 and h == 0), stop=(cc == ncc - 1 and h == 3)),
                             reads=[("pT", pb), "cst"], writes=[("ps", UB)])
                attn_tile([(kcmpT[:, cc * 128:(cc + 1) * 128], rq, ["cmp", ("qnb", qb)]), (identb[:], Bc[:, bi, :], ["cst", "cst0"])],
                          vcmp[:, cc, 0:129], (OA0, OA1), cc == 0, cc == ncc - 1, extra)
            finish_branch((OA0, OA1), 0, gs, ab, True)
            P.op("dve", lambda e: e.tensor_scalar(out=score[:, 0:NS], in0=ps[UB][:, 0:NS], scalar1=rc4[:, 0:1], scalar2=None, op0=ALU.mult),
                 reads=[("ps", UB), "rc4"], writes=["score"])
            for h in range(1, 4):
                P.op("dve", lambda e, h=h: e.scalar_tensor_tensor(out=score[:, 0:NS], in0=ps[UB][:, h * 128:h * 128 + NS], scalar=rc4[:, h:h + 1],
                                                                 in1=score[:, 0:NS], op0=ALU.mult, op1=ALU.add),
                     reads=[("ps", UB), "rc4", "score"], writes=["score"])
            P.op("dve", lambda e, j=j: e.tensor_tensor(out=score[:, 0:NS], in0=score[:, 0:NS], in1=wsel[:, 128 - 2 * j:128 - 2 * j + NS], op=ALU.add),
                 reads=["score", "cst"], writes=["score"])
            P.op("dve", lambda e: e.tensor_scalar(out=score[:, 0:1], in0=score[:, 0:1], scalar1=1e4, scalar2=None, op0=ALU.add),
                 reads=["score"], writes=["score"])
            if NS > 16:
                P.op("dve", lambda e: e.max(out=m8a[:], in_=score[:, 0:NS]), reads=["score"], writes=["m8a"])
                P.op("dve", lambda e: e.match_replace(out=sc2[:, 0:NS], in_to_replace=m8a[:], in_values=score[:, 0:NS], imm_value=-3e38),
                     reads=["score", "m8a"], writes=["sc2"])
                P.op("dve", lambda e: e.max(out=m8b[:], in_=sc2[:, 0:NS]), reads=["sc2"], writes=["m8b"])
                P.op("dve", lambda e: e.tensor_scalar(out=Msel[:, 0:NS], in0=score[:, 0:NS], scalar1=m8b[:, 7:8], scalar2=None, op0=ALU.is_ge),
                     reads=["score", "m8b"], writes=["Msel"])
            else:
                P.op("dve", lambda e: e.memset(Msel[:, 0:NS], 1.0), writes=["Msel"])
            P.op("pe", lambda e: e.transpose(out=ps[MB][0:NS, 0:128], in_=Msel[:, 0:NS], identity=ident[:]), reads=["Msel", "cst0"], writes=[("ps", MB)])
            P.op("dve", lambda e: e.tensor_scalar(out=pen4[0:NS], in0=ps[MB][0:NS, 0:128].unsqueeze(1).to_broadcast([NS, 4, 128]), scalar1=-1.0, scalar2=BIG,
                                                  op0=ALU.add, op1=ALU.mult), reads=[("ps", MB)], writes=["pen4"])
            penf = pen4[0:NS].rearrange("p h t -> p (h t)")
            for kc in range(j + 1):
                dl = j - kc
                bi = dl if dl < 8 else 8
                attn_tile([(ksn[:, kc * 128:(kc + 1) * 128], rq, ["ksn", ("qnb", qb)]), (Ew[0:NS, kc * 128:(kc + 1) * 128], penf, ["cst", "pen4"]),
                           (identb[:], Bs[:, bi, :], ["cst", "cst0"])], vsb[:, kc, 0:129], (OB0, OB1), kc == 0, kc == j)
            finish_branch((OB0, OB1), 1, gs, ab, False)
            k0 = max(0, j - 4)
            for kc in range(k0, j + 1):
                dl = j - kc
                bi = dl if dl < 4 else 9
                attn_tile([(kwn[:, kc * 128:(kc + 1) * 128], rq, ["ksn", ("qnb", qb)]), (identb[:], Bs[:, bi, :], ["cst", "cst0"])],
                          vwb[:, kc, 0:129], (OA0, OA1), kc == k0, kc == j)
            finish_branch((OA0, OA1), 2, gs, ab, False)
            P.dma("sp", yb_d[r0:r0 + 128, :], acc[ab][:], reads=[("acc", ab, h) for h in range(4)], writes=[("yb", j)])


def emit_nsa_compress(nc, P, st, S, zfm_d, din, ones, epst, kcmpT, vcmp):
    NCMP = (S - 32) // 16 + 1
    NCC = (NCMP + 127) // 128
    sb = lambda name, shape, dt=F32: st.enter_context(nc.sbuf_tensor(name, shape, dt))
    stg = [sb("c_stg%d" % i, [128, 2048]) for i in range(2)]
    kcb = sb("c_kcb", [128, S], BF16)
    vcb = sb("c_vcb", [128, S], BF16)
    w1 = [sb("c_w1_%d" % i, [128, 32, 128], BF16) for i in range(2)]
    w2 = [sb("c_w2_%d" % i, [128, 128], BF16) for i in range(2)]
    posb = sb("c_pos", [128, 32], BF16)
    kg = sb("c_kg", [128, 1])
    bia = sb("c_bia", [128, 2])
    xb = sb("c_xb", [128, 512])
    x2 = sb("c_x2", [128, 512])
    sg = sb("c_sg", [128, 512])
    hid = [sb("c_hid%d" % i, [128, 512], BF16) for i in range(2)]
    ps = [st.enter_context(nc.psum_tensor("c_ps%d" % i, [128, 512], F32)) for i in range(4)]
    cnt = {"stg": 0}

    def load_cast(dst, src, shape_view, n, eng="pool"):
        b = cnt["stg"] % 2
        cnt["stg"] += 1
        sv = stg[b][:, 0:n] if shape_view is None else stg[b][:, 0:n].rearrange(shape_view[0], **shape_view[1])
        P.dma("sp", sv, src, writes=[("stg", b)])
        P.op(eng, lambda e: e.tensor_copy(out=dst, in_=sv), reads=[("stg", b)], writes=["cw"])

    n = min(2048, S)
    for (row0, dst) in ((FM_KC, kcb), (FM_VC, vcb)):
        for i in range(S // n):
            load_cast(dst[:, i * n:(i + 1) * n], zfm_d[row0:row0 + 128, i * n:(i + 1) * n], None, n, "dve" if i % 2 else "pool")
    for wi, nm in enumerate(("k", "v")):
        for q in range(2):
            load_cast(w1[wi][:, q * 16:(q + 1) * 16, :], din["c%sw1" % nm][:, q * 16:(q + 1) * 16, :], ("p (l o) -> p l o", dict(o=128)), 2048)
        load_cast(w2[wi][:], din["c%sw2" % nm], None, 128)
    load_cast(posb[:], din["posT"], None, 32)
    P.dma("sp", kg[:], din["kg"], writes=["cw"])
    P.op("dve", lambda e: e.memset(kcmpT[:], 0.0), writes=["cmp"])
    P.op("dve", lambda e: e.memset(vcmp[:], 0.0), writes=["cmp"])
    P.op("dve", lambda e: e.memset(vcmp[:, :, 128:130], 1.0), writes=["cmp"])
    for wi, src in enumerate((kcb, vcb)):
        for l in range(32):
            P.op("pe", lambda e, l=l, wi=wi: e.matmul(ps[0][:, 0:1], lhsT=w1[wi][:, l, :], rhs=posb[:, l:l + 1], start=(l == 0), stop=(l == 31)),
                 reads=["cw"], writes=[("cps", 0)])
        P.op("dve", lambda e, wi=wi: e.tensor_copy(out=bia[:, wi:wi + 1], in_=ps[0][:, 0:1]), reads=[("cps", 0)], writes=["bia"])
        for l in range(32):
            P.op("pe", lambda e, l=l, wi=wi, src=src: e.matmul(ps[1][:, 0:NCMP], lhsT=w1[wi][:, l, :], rhs=src[:, l:l + 16 * (NCMP - 1) + 1:16],
                                                              start=(l == 0), stop=(l == 31)), reads=["cw"], writes=[("cps", 1)])
        P.op("dve", lambda e, wi=wi: e.tensor_scalar(out=xb[:, 0:NCMP], in0=ps[1][:, 0:NCMP], scalar1=bia[:, wi:wi + 1], scalar2=None, op0=ALU.add),
             reads=[("cps", 1), "bia"], writes=["xb"])
        P.op("dve", lambda e: e.tensor_tensor(out=x2[:, 0:NCMP], in0=xb[:, 0:NCMP], in1=xb[:, 0:NCMP], op=ALU.mult), reads=["xb"], writes=["x2"])
        P.op("dve", lambda e: e.tensor_scalar(out=x2[:, 0:NCMP], in0=x2[:, 0:NCMP], scalar1=0.044715, scalar2=1.0, op0=ALU.mult, op1=ALU.add),
             reads=["x2"], writes=["x2"])
        P.op("dve", lambda e: e.tensor_tensor(out=x2[:, 0:NCMP], in0=x2[:, 0:NCMP], in1=xb[:, 0:NCMP], op=ALU.mult), reads=["x2", "xb"], writes=["x2"])
        P.op("act", lambda e: e.activation(out=sg[:, 0:NCMP], in_=x2[:, 0:NCMP], func=AF.Sigmoid, scale=1.5957691216), reads=["x2"], writes=["sg"])
        P.op("dve", lambda e, wi=wi: e.memset(hid[wi][:], 0.0), writes=[("hid", wi)])
        P.op("dve", lambda e, wi=wi: e.tensor_tensor(out=hid[wi][:, 0:NCMP], in0=xb[:, 0:NCMP], in1=sg[:, 0:NCMP], op=ALU.mult),
             reads=["xb", "sg"], writes=[("hid", wi)])
        if wi == 0:
            P.op("pe", lambda e: e.matmul(ps[2][:, 0:NCMP], lhsT=w2[0][:], rhs=hid[0][:, 0:NCMP], start=True, stop=True), reads=["cw", ("hid", 0)],
                 writes=[("cps", 2)])
            P.op("act", lambda e: e.activation(out=xb[:, 0:NCMP], in_=ps[2][:, 0:NCMP], func=AF.Square), reads=[("cps", 2)], writes=["xb"])
            P.op("pe", lambda e: e.matmul(ps[3][:, 0:NCMP], lhsT=ones[:], rhs=xb[:, 0:NCMP], start=True, stop=True), reads=["xb", "cst0"], writes=[("cps", 3)])
            P.op("act", lambda e: e.activation(out=sg[:, 0:NCMP], in_=ps[3][:, 0:NCMP], func=AF.Sqrt, scale=1.0 / 128.0, bias=epst[:, 0:1]),
                 reads=[("cps", 3)], writes=["sg"])
            P.op("dve", lambda e: e.reciprocal(out=sg[:, 0:NCMP], in_=sg[:, 0:NCMP]), reads=["sg"], writes=["sg"])
            P.op("dve", lambda e: e.scalar_tensor_tensor(out=kcmpT[:, 0:NCMP], in0=ps[2][:, 0:NCMP], scalar=kg[:, 0:1], in1=sg[:, 0:NCMP],
                                                        op0=ALU.mult, op1=ALU.mult), reads=[("cps", 2), "sg", "cw"], writes=["cmp"])
        else:
            for cc in range(NCC):
                P.op("pe", lambda e, cc=cc: e.matmul(ps[2][:, 0:128], lhsT=hid[1][:, cc * 128:(cc + 1) * 128], rhs=w2[1][:], start=True, stop=True),
                     reads=["cw", ("hid", 1)], writes=[("cps", 2)])
                P.op("dve", lambda e, cc=cc: e.tensor_copy(out=vcmp[:, cc, 0:128], in_=ps[2][:, 0:128]), reads=[("cps", 2)], writes=["cmp"])


NE = 32
DFF = 1536
CAP = 768


def emit_cast_to_dram(nc, P, st, src_d, dst_d, n_outer, inner, tagp):
    stg = [st.enter_context(nc.sbuf_tensor("%s_s%d" % (tagp, i), [128, inner], F32)) for i in range(2)]
    stb = [st.enter_context(nc.sbuf_tensor("%s_b%d" % (tagp, i), [128, inner], BF16)) for i in range(2)]
    for i in range(n_outer):
        b = i % 2
        P.dma("sp", stg[b][:], src_d[i], writes=[(tagp + "s", b)])
        P.op("pool" if i % 2 else "dve", lambda e, b=b: e.tensor_copy(out=stb[b][:], in_=stg[b][:]), reads=[(tagp + "s", b)], writes=[(tagp + "b", b)])
        P.dma("act", dst_d[i], stb[b][:], reads=[(tagp + "b", b)], writes=[(tagp + "d", i)])


def emit_gelu(P, eng_v, x, tmp, sgt, out, n, rk, wk, tag):
    P.op(eng_v, lambda e: e.tensor_tensor(out=tmp, in0=x, in1=x, op=ALU.mult), reads=rk, writes=[tag + "tmp"])
    P.op(eng_v, lambda e: e.tensor_scalar(out=tmp, in0=tmp, scalar1=0.044715, scalar2=1.0, op0=ALU.mult, op1=ALU.add), reads=[tag + "tmp"], writes=[tag + "tmp"])
    P.op(eng_v, lambda e: e.tensor_tensor(out=tmp, in0=tmp, in1=x, op=ALU.mult), reads=[tag + "tmp"] + list(rk), writes=[tag + "tmp"])
    P.op("act", lambda e: e.activation(out=sgt, in_=tmp, func=AF.Sigmoid, scale=1.5957691216), reads=[tag + "tmp"], writes=[tag + "sg"])
    P.op(eng_v, lambda e: e.tensor_tensor(out=out, in0=x, in1=sgt, op=ALU.mult), reads=list(rk) + [tag + "sg"], writes=wk)


def emit_gmlp(nc, P, st, T, zgu_d, zgv_d, ycT_d, din, tri, ident, epst):
    sb = lambda name, shape, dt=F32: st.enter_context(nc.sbuf_tensor(name, shape, dt))
    wst = sb("g_wst", [128, 8, 128])
    wsb = sb("g_wsb", [128, 8, 128], BF16)
    bT = sb("g_bT", [128, 8, 128])
    gng = sb("g_gng", [128, 1024])
    gv = [sb("g_gv%d" % i, [128, 1024]) for i in range(2)]
    tmp = sb("g_tmp", [128, 1024])
    sgt = sb("g_sgt", [128, 1024])
    vnb = [sb("g_vnb%d" % i, [128, 1024], BF16) for i in range(2)]
    gu = [sb("g_gu%d" % i, [128, 8, 128]) for i in range(2)]
    tmpu = sb("g_tmpu", [128, 8, 128])
    sgu = sb("g_sgu", [128, 8, 128])
    yc = [sb("g_yc%d" % i, [128, 8, 128], BF16) for i in range(2)]
    stt = [sb("g_stt%d" % i, [128, 8]) for i in range(2)]
    ps = [st.enter_context(nc.psum_tensor("g_ps%d" % i, [128, 512], F32)) for i in range(4)]
    P.dma("sp", wst[:], din["wsT"], writes=["wst"])
    P.dma("sp", bT[:], din["gbT"], writes=["gcst"])
    P.dma("sp", gng[:], din["gng"], writes=["gcst"])
    P.op("dve", lambda e: e.tensor_tensor(out=wsb[:], in0=wst[:], in1=tri[:].unsqueeze(1).to_broadcast([128, 8, 128]), op=ALU.mult),
         reads=["wst", "cst0"], writes=["wsb"])
    for c in range(T // 128):
        b = c % 2
        t0 = c * 128
        P.dma("sp", gv[b][:], zgv_d[t0:t0 + 128, :], reads=[("zg", "v")], writes=[("gv", b)])
        P.dma("sp", gu[b][:], zgu_d[:, t0:t0 + 128].rearrange("(g p) t -> p g t", p=128), reads=[("zg", "u")], writes=[("gu", b)])
        emit_gelu(P, "dve", gv[b][:], tmp[:], sgt[:], gv[b][:], 1024, [("gv", b)], [("gv", b)], "gv")
        P.op("act", lambda e, b=b: e.activation(out=tmp[:], in_=gv[b][:], func=AF.Copy, accum_out=stt[b][:, 0:1]), reads=[("gv", b)], writes=["gvtmp", ("stt", b, 0)])
        P.op("dve", lambda e, b=b: e.tensor_scalar(out=stt[b][:, 1:2], in0=stt[b][:, 0:1], scalar1=-1.0 / 1024.0, scalar2=None, op0=ALU.mult),
             reads=[("stt", b, 0)], writes=[("stt", b, 1)])
        P.op("act", lambda e, b=b: e.activation(out=tmp[:], in_=gv[b][:], func=AF.Square, bias=stt[b][:, 1:2], scale=1.0, accum_out=stt[b][:, 2:3]),
             reads=[("gv", b), ("stt", b, 1)], writes=["gvtmp", ("stt", b, 2)])
        P.op("act", lambda e, b=b: e.activation(out=stt[b][:, 3:4], in_=stt[b][:, 2:3], func=AF.Sqrt, scale=1.0 / 1024.0, bias=epst[:, 0:1]),
             reads=[("stt", b, 2)], writes=[("stt", b, 3)])
        P.op("dve", lambda e, b=b: e.reciprocal(out=stt[b][:, 4:5], in_=stt[b][:, 3:4]), reads=[("stt", b, 3)], writes=[("stt", b, 4)])
        P.op("dve", lambda e, b=b: e.tensor_scalar(out=tmp[:], in0=gv[b][:], scalar1=stt[b][:, 1:2], scalar2=stt[b][:, 4:5], op0=ALU.add, op1=ALU.mult),
             reads=[("gv", b), ("stt", b, 1), ("stt", b, 4)], writes=["gvtmp"])
        P.op("pool", lambda e, b=b: e.tensor_tensor(out=vnb[b][:], in0=tmp[:], in1=gng[:], op=ALU.mult), reads=["gvtmp", "gcst"], writes=[("vnb", b)])
        emit_gelu(P, "pool", gu[b][:], tmpu[:], sgu[:], gu[b][:], 1024, [("gu", b)], [("gu", b)], "gu")
        for g in range(8):
            pb = (g // 4) + 2 * (c % 2)
            P.op("pe", lambda e, g=g, pb=pb, b=b: e.matmul(ps[pb][:, (g % 4) * 128:(g % 4 + 1) * 128], lhsT=vnb[b][:, g * 128:(g + 1) * 128], rhs=wsb[:, g, :],
                                                         start=True, stop=True), reads=[("vnb", b), "wsb"], writes=[("gps", pb)])
        for half in range(2):
            pb = half + 2 * (c % 2)
            P.op("dve", lambda e, half=half, pb=pb: e.tensor_tensor(out=tmpu[:, half * 4:(half + 1) * 4, :], in0=ps[pb][:].rearrange("p (g t) -> p g t", t=128),
                                                                  in1=bT[:, half * 4:(half + 1) * 4, :], op=ALU.add), reads=[("gps", pb), "gcst"], writes=["gutmp"])
        P.op("dve", lambda e, b=b: e.tensor_tensor(out=yc[b][:], in0=tmpu[:], in1=gu[b][:], op=ALU.mult), reads=["gutmp", ("gu", b)], writes=[("yc", b)])
        P.dma("sp", ycT_d[:, :, t0:t0 + 128], yc[b][:], reads=[("yc", b)], writes=[("ycT", c)])


def emit_merge(nc, P, st, T, x_d, hT_d, yaT_d, ybT_d, ycT_d, wg_b, wb_b, wo_b, g1b_d, x1_d):
    sb = lambda name, shape, dt=F32: st.enter_context(nc.sbuf_tensor(name, shape, dt))
    wo = sb("e_wo", [128, 16, 2048], BF16)
    g1b = sb("e_g1b", [128, 2048])
    ht = sb("e_ht", [128, 16, 512], BF16)
    yst = [sb("e_yst%d" % i, [128, 8, 512]) for i in range(1)]
    yt = [sb("e_yt%d" % i, [128, 8, 512], BF16) for i in range(3)]
    wg = [sb("e_wg%d" % i, [128, 3, 16, 128], BF16) for i in range(2)]
    wb = [sb("e_wb%d" % i, [128, 3, 8, 128], BF16) for i in range(2)]
    sg = [sb("e_sg%d" % i, [128, 512]) for i in range(3)]
    mt = sb("e_mt", [128, 512])
    mT = sb("e_mT", [128, 16, 512], BF16)
    xt = [sb("e_xt%d" % i, [128, 2048]) for i in range(1)]
    ps = [st.enter_context(nc.psum_tensor("e_ps%d" % i, [128, 512], F32)) for i in range(8)]
    P.dma("sp", wo[:], wo_b, reads=["wo_b"], writes=["wo"])
    P.dma("sp", g1b[:], g1b_d, writes=["g1b"])
    wi = 0
    for t in range(T // 512):
        t0 = t * 512
        P.dma("sp", ht[:], hT_d[:, :, t0:t0 + 512], reads=[("hT_d", t)], writes=["ht"])
        for i, src in enumerate((yaT_d, ybT_d)):
            P.dma("sp", yst[0][:], src[:, t0:t0 + 512].rearrange("(k p) t -> p k t", p=128), writes=[("yst", 0)])
            P.op("pool", lambda e, i=i: e.tensor_copy(out=yt[i][:], in_=yst[0][:]), reads=[("yst", 0)], writes=[("yt", i)])
        P.dma("sp", yt[2][:], ycT_d[:, :, t0:t0 + 512], reads=[("ycT", c) for c in range(t * 4, t * 4 + 4)], writes=[("yt", 2)])
        for f in range(16):
            b = wi % 2
            wi += 1
            P.dma("act", wg[b][:], wg_b[f], reads=["wg_b"], writes=[("wg", b)])
            P.dma("act", wb[b][:], wb_b[f], reads=["wb_b"], writes=[("wb", b)])
            for i in range(3):
                for k in range(16):
                    P.op("pe", lambda e, i=i, k=k, b=b: e.matmul(ps[i][:], lhsT=wg[b][:, i, k, :], rhs=ht[:, k, :], start=(k == 0), stop=(k == 15)),
                         reads=[("wg", b), "ht"], writes=[("eps", i)])
                for k in range(8):
                    P.op("pe", lambda e, i=i, k=k, b=b: e.matmul(ps[3 + i][:], lhsT=wb[b][:, i, k, :], rhs=yt[i][:, k, :], start=(k == 0), stop=(k == 7)),
                         reads=[("wb", b), ("yt", i)], writes=[("eps", 3 + i)])
            for i in range(3):
                P.op("act", lambda e, i=i: e.activation(out=sg[i][:], in_=ps[i][:], func=AF.Sigmoid), reads=[("eps", i)], writes=[("sg", i)])
            P.op("dve", lambda e: e.tensor_tensor(out=mt[:], in0=sg[0][:], in1=ps[3][:], op=ALU.mult), reads=[("sg", 0), ("eps", 3)], writes=["mt"])
            P.op("dve", lambda e: e.tensor_tensor(out=sg[1][:], in0=sg[1][:], in1=ps[4][:], op=ALU.mult), reads=[("sg", 1), ("eps", 4)], writes=[("sg", 1)])
            P.op("dve", lambda e: e.tensor_tensor(out=sg[2][:], in0=sg[2][:], in1=ps[5][:], op=ALU.mult), reads=[("sg", 2), ("eps", 5)], writes=[("sg", 2)])
            P.op("pool", lambda e: e.tensor_tensor(out=mt[:], in0=mt[:], in1=sg[1][:], op=ALU.add), reads=["mt", ("sg", 1)], writes=["mt"])
            P.op("pool", lambda e, f=f: e.tensor_tensor(out=mT[:, f, :], in0=mt[:], in1=sg[2][:], op=ALU.add), reads=["mt", ("sg", 2)], writes=["mT"])
        for sub in range(4):
            xb = 0
            r0 = t0 + sub * 128
            P.dma("sp", xt[xb][:], x_d[r0:r0 + 128, :], writes=[("ext", xb)])
            for cg in range(4):
                pb = 6 + cg % 2
                for k in range(16):
                    P.op("pe", lambda e, k=k, cg=cg, pb=pb, sub=sub: e.matmul(ps[pb][:], lhsT=mT[:, k, sub * 128:(sub + 1) * 128], rhs=wo[:, k, cg * 512:(cg + 1) * 512],
                                                                             start=(k == 0), stop=(k == 15)), reads=["mT", "wo"], writes=[("eps", pb)])
                P.op("dve", lambda e, cg=cg, pb=pb: e.tensor_tensor(out=mt[:], in0=ps[pb][:], in1=g1b[:, cg * 512:(cg + 1) * 512], op=ALU.mult),
                     reads=[("eps", pb), "g1b"], writes=["mt"])
                P.op("pool", lambda e, cg=cg, xb=xb: e.tensor_tensor(out=xt[xb][:, cg * 512:(cg + 1) * 512], in0=xt[xb][:, cg * 512:(cg + 1) * 512], in1=mt[:], op=ALU.add),
                     reads=["mt", ("ext", xb)], writes=[("ext", xb)])
            P.dma("sp", x1_d[r0:r0 + 128, :], xt[xb][:], reads=[("ext", xb)], writes=["x1_d"])


def emit_route(nc, P, st, T, x1_d, modv2, g2n, ident, tri, ones, epst, h2T_d, xn_d, din, pm_d, slot_d, idx_all, ix, cnt_d, ne, blk, nblk):
    sb = lambda name, shape, dt=F32: st.enter_context(nc.sbuf_tensor(name, shape, dt))
    NT = T // 128
    NSC = blk // 128
    rw = sb("r_rw", [128, 16, ne])
    rbt = sb("r_rb", [128, ne])
    htf = [sb("r_htf%d" % i, [128, 16, 128]) for i in range(2)]
    lg = [sb("r_lg%d" % i, [128, ne]) for i in range(2)]
    m8 = [sb("r_m8%d" % i, [128, 8]) for i in range(2)]
    nmx = [sb("r_nmx%d" % i, [128, 1]) for i in range(2)]
    msk = [sb("r_msk%d" % i, [128, ne]) for i in range(2)]
    ex = [sb("r_ex%d" % i, [128, ne]) for i in range(2)]
    dn = [sb("r_dn%d" % i, [128, 2]) for i in range(2)]
    pm = [sb("r_pm%d" % i, [128, ne]) for i in range(2)]
    PmT = sb("r_PmT", [ne, 1, T])
    c0 = sb("r_c0", [ne, 1, T])
    c1 = sb("r_c1", [ne, 1, T])
    psr = [st.enter_context(nc.psum_tensor("r_ps%d" % i, [128, 512], F32)) for i in range(3)]
    P.dma("sp", rw[:], din["rw"], writes=["rw"])
    P.dma("sp", rbt[:], din["rb"], writes=["rw"])

    def hook(kind, b, k, pb, ps, scl, r0):
        if kind == "chunk":
            P.op("dve", lambda e: e.tensor_scalar(out=htf[b][:, k, :], in0=ps[pb][:, (k % 4) * 128:(k % 4 + 1) * 128], scalar1=scl[:, k:k + 1],
                                                  scalar2=modv2[:, 0, k:k + 1], op0=ALU.mult, op1=ALU.add),
                 reads=[("n2ps", pb), "n2scl", "modv"], writes=[("htf", b)])
            return
        for k_ in range(16):
            P.op("pe", lambda e, k_=k_: e.matmul(psr[0][:, 0:ne], lhsT=htf[b][:, k_, :], rhs=rw[:, k_, :], start=(k_ == 0), stop=(k_ == 15)),
                 reads=[("htf", b), "rw"], writes=[("rps", 0)])
        P.op("dve", lambda e: e.tensor_tensor(out=lg[b][:], in0=psr[0][:, 0:ne], in1=rbt[:], op=ALU.add), reads=[("rps", 0), "rw"], writes=[("lg", b)])
        P.op("dve", lambda e: e.max(out=m8[b][:], in_=lg[b][:]), reads=[("lg", b)], writes=[("m8", b)])
        P.op("dve", lambda e: e.tensor_scalar(out=nmx[b][:], in0=m8[b][:, 0:1], scalar1=-1.0, scalar2=None, op0=ALU.mult), reads=[("m8", b)], writes=[("nmx", b)])
        P.op("dve", lambda e: e.tensor_scalar(out=msk[b][:], in0=lg[b][:], scalar1=m8[b][:, 3:4], scalar2=None, op0=ALU.is_ge), reads=[("lg", b), ("m8", b)],
             writes=[("msk", b)])
        P.op("act", lambda e: e.activation(out=ex[b][:], in_=lg[b][:], func=AF.Exp, bias=nmx[b][:, 0:1]), reads=[("lg", b), ("nmx", b)], writes=[("ex", b)])
        P.op("dve", lambda e: e.tensor_tensor(out=ex[b][:], in0=ex[b][:], in1=msk[b][:], op=ALU.mult), reads=[("ex", b), ("msk", b)], writes=[("ex", b)])
        P.op("dve", lambda e: e.reduce_sum(out=dn[b][:, 0:1], in_=ex[b][:], axis=AX.X), reads=[("ex", b)], writes=[("dn", b)])
        P.op("dve", lambda e: e.reciprocal(out=dn[b][:, 1:2], in_=dn[b][:, 0:1]), reads=[("dn", b)], writes=[("dn", b)])
        P.op("dve", lambda e: e.tensor_scalar(out=pm[b][:], in0=ex[b][:], scalar1=dn[b][:, 1:2], scalar2=None, op0=ALU.mult), reads=[("ex", b), ("dn", b)],
             writes=[("pm", b)])
        P.dma("act", pm_d[r0:r0 + 128, :], pm[b][:], reads=[("pm", b)], writes=[("pm_d", r0)])
        P.op("pe", lambda e: e.transpose(out=psr[1][0:ne, 0:128], in_=pm[b][:], identity=ident[:]), reads=[("pm", b), "cst0"], writes=[("rps", 1)])
        P.op("act", lambda e: e.copy(out=PmT[:, 0, r0:r0 + 128], in_=psr[1][0:ne, 0:128]), reads=[("rps", 1)], writes=["PmT"])

    emit_norm_to_hT(nc, P, st, x1_d, T, modv2, g2n, ident, epst, h2T_d, tagp="n2", xn_d=xn_d, hook=hook, xkey="x1_d", dbl=False)
    P.op("dve", lambda e: e.tensor_scalar(out=c0[:], in0=PmT[:], scalar1=0.0, scalar2=None, op0=ALU.is_gt), reads=["PmT"], writes=["c0"])
    P.op("pool", lambda e: e.tensor_copy(out=PmT[:], in_=c0[:]), reads=["c0"], writes=["PmT"])
    r = _scan_free(P, "dve", [c0, c1], ["c0", "c1"], T, ALU.add)
    cum = [c0, c1][r]
    ck = ["c0", "c1"][r]
    oth = [c0, c1][1 - r]
    ok = ["c0", "c1"][1 - r]
    P.dma("sp", cnt_d, cum[:, 0, T - 1:T], reads=[ck], writes=["cnt_d"])
    BLKF = float(blk)
    sm = sb("r_sm", [ne, 8])
    J = T // blk
    thr = sb("r_thr", [ne, J])
    thc = sb("r_thc", [ne, J])
    P.dma("sp", thr[:], din["thr"], writes=["thr"])
    P.op("dve", lambda e: e.tensor_scalar(out=thc[:], in0=thr[:], scalar1=cum[:, 0, T - 1:T], scalar2=None, op0=ALU.is_lt), reads=[ck, "thr"], writes=["thc"])
    P.op("dve", lambda e: e.reduce_sum(out=sm[:, 0:1], in_=thc[:], axis=AX.X), reads=["thc"], writes=["sm0"])
    P.op("dve", lambda e: e.tensor_scalar(out=sm[:, 2:3], in0=sm[:, 0:1], scalar1=BLKF, scalar2=None, op0=ALU.mult), reads=["sm0"], writes=["sm2"])
    P.op("pe", lambda e: e.matmul(psr[1][0:ne, 0:1], lhsT=tri[0:ne, 0:ne], rhs=sm[:, 2:3], start=True, stop=True), reads=["sm2", "cst0"], writes=[("rps", 1)])
    P.op("dve", lambda e: e.tensor_copy(out=sm[:, 3:4], in_=psr[1][0:ne, 0:1]), reads=[("rps", 1)], writes=["sm3"])
    P.op("dve", lambda e: e.tensor_tensor(out=sm[:, 4:5], in0=sm[:, 3:4], in1=sm[:, 2:3], op=ALU.subtract), reads=["sm3", "sm2"], writes=["sm4"])
    P.op("dve", lambda e: e.tensor_tensor(out=oth[:], in0=cum[:], in1=PmT[:], op=ALU.subtract), reads=[ck, "PmT"], writes=[ok])
    P.op("dve", lambda e: e.tensor_scalar(out=oth[:], in0=oth[:], scalar1=sm[:, 4:5], scalar2=None, op0=ALU.add), reads=[ok, "sm4"], writes=[ok])
    slt = [sb("r_slt%d" % i, [128, ne]) for i in range(2)]
    for t in range(NT):
        b = t % 2
        P.op("pe", lambda e, t=t: e.transpose(out=psr[1][:, 0:ne], in_=oth[:, 0, t * 128:(t + 1) * 128], identity=ident[0:ne, 0:ne]), reads=[ok, "cst0"],
             writes=[("rps", 1)])
        P.op("dve", lambda e, b=b: e.tensor_copy(out=slt[b][:], in_=psr[1][:, 0:ne]), reads=[("rps", 1)], writes=[("slt", b)])
        P.dma("act", slot_d[t * 128:(t + 1) * 128, :], slt[b][:], reads=[("slt", b)], writes=[("slot_d", t)])
    rowt = sb("r_rowt", [1, 128])
    pendb = sb("r_pendb", [128, ne])
    bst = sb("r_bst", [128, 1])
    pidx = sb("r_pidx", [ne, 1])
    bex = sb("r_bex", [128, 2])
    rv = sb("r_rv", [128, nblk * NSC])
    P.dma("sp", bst[:], din["bst"], writes=["bst"])
    P.dma("sp", pidx[:], din["pidx"], writes=["pidx"])
    P.dma("sp", rv[:], din["rv"], writes=["rv"])
    P.op("pe", lambda e: e.transpose(out=psr[1][0:1, 0:ne], in_=sm[:, 3:4], identity=ident[0:ne, 0:ne]), reads=["sm3", "cst0"], writes=[("rps", 1)])
    P.op("dve", lambda e: e.tensor_copy(out=rowt[:, 0:ne], in_=psr[1][0:1, 0:ne]), reads=[("rps", 1)], writes=["rowt"])
    P.op("pe", lambda e: e.matmul(psr[1][:, 0:ne], lhsT=ones[0:1, :], rhs=rowt[:, 0:ne], start=True, stop=True), reads=["rowt", "cst0"], writes=[("rps", 1)])
    P.op("dve", lambda e: e.tensor_scalar(out=pendb[:], in0=psr[1][:, 0:ne], scalar1=bst[:, 0:1], scalar2=None, op0=ALU.is_le), reads=[("rps", 1), "bst"],
         writes=["pendb"])
    P.op("dve", lambda e: e.reduce_sum(out=bex[:, 0:1], in_=pendb[:], axis=AX.X), reads=["pendb"], writes=["bex"])
    P.op("dve", lambda e: e.tensor_scalar(out=bex[:, 0:1], in0=bex[:, 0:1], scalar1=float(ne - 1), scalar2=None, op0=ALU.min), reads=["bex"], writes=["bex"])
    P.op("pe", lambda e: e.transpose(out=psr[1][0:1, 0:128], in_=bex[:, 0:1], identity=ident[:]), reads=["bex", "cst0"], writes=[("rps", 1)])
    P.op("dve", lambda e: e.tensor_copy(out=rowt[:], in_=psr[1][0:1, 0:128]), reads=[("rps", 1)], writes=["rowt"])
    pcol = sb("r_pcol", [128, 1])
    P.dma("sp", pcol[:], din["pcol"], writes=["pcol"])
    bas = sb("r_bas", [128, 128])
    tix = sb("r_tix", [128, 128])
    P.op("pe", lambda e: e.matmul(psr[2][:, 0:128], lhsT=ones[0:1, :], rhs=rowt[:], start=True, stop=True), reads=["rowt", "cst0"], writes=[("rps", 2)])
    P.op("dve", lambda e: e.tensor_copy(out=ix["b2"][:], in_=psr[2][:, 0:nblk]), reads=[("rps", 2)], writes=["ixb2"])
    P.op("dve", lambda e: e.tensor_scalar(out=bas[:], in0=psr[2][:, 0:128], scalar1=128.0, scalar2=pcol[:, 0:1], op0=ALU.mult, op1=ALU.add),
         reads=[("rps", 2), "pcol"], writes=["bas"])
    P.op("dve", lambda e: e.tensor_copy(out=ix["b1"][:], in_=bas[:, 0:nblk]), reads=["bas"], writes=["ixb1"])
    for pc in range(6):
        P.op("dve", lambda e, pc=pc: e.tensor_scalar(out=tix[:], in0=bas[:], scalar1=6.0, scalar2=float(pc), op0=ALU.mult, op1=ALU.add), reads=["bas"], writes=["tix"])
        P.op("dve", lambda e, pc=pc: e.tensor_copy(out=ix["w1"][:, :, pc], in_=tix[:, 0:nblk]), reads=["tix"], writes=["ixw1"])
    for pc in range(4):
        P.op("dve", lambda e, pc=pc: e.tensor_scalar(out=tix[:], in0=bas[:], scalar1=4.0, scalar2=float(pc), op0=ALU.mult, op1=ALU.add), reads=["bas"], writes=["tix"])
        P.op("dve", lambda e, pc=pc: e.tensor_copy(out=ix["w2"][:, :, pc], in_=tix[:, 0:nblk]), reads=["tix"], writes=["ixw2"])
    SelB = sb("r_SelB", [ne, 128])
    P.op("pe", lambda e: e.matmul(psr[1][0:ne, 0:128], lhsT=ones[0:1, 0:ne], rhs=rowt[:], start=True, stop=True), reads=["rowt", "cst0"], writes=[("rps", 1)])
    P.op("dve", lambda e: e.tensor_scalar(out=SelB[:], in0=psr[1][0:ne, 0:128], scalar1=pidx[:, 0:1], scalar2=None, op0=ALU.is_equal), reads=[("rps", 1), "pidx"],
         writes=["SelB"])
    bcs = [sb("r_bcs%d" % i, [128, T]) for i in range(2)]
    psb_ = [sb("r_psb%d" % i, [128, 1]) for i in range(2)]
    junk = sb("r_junk", [128, T])
    cntf = sb("r_cntf", [128, nblk, NSC])
    for i in range(nblk):
        b = i % 2
        sel = SelB[:, i:i + 1].to_broadcast([ne, 128])
        for pc in range(T // 512):
            pb = pc % 2
            P.op("pe", lambda e, sel=sel, pc=pc, pb=pb: e.matmul(psr[1 + pb][:], lhsT=sel, rhs=cum[:, 0, pc * 512:(pc + 1) * 512], start=True, stop=True),
                 reads=[ck, "SelB"], writes=[("rps", 1 + pb)])
            P.op("act", lambda e, b=b, pc=pc, pb=pb: e.copy(out=bcs[b][:, pc * 512:(pc + 1) * 512], in_=psr[1 + pb][:]), reads=[("rps", 1 + pb)], writes=[("bcs", b)])
        P.op("pe", lambda e, sel=sel: e.matmul(psr[0][:, 0:1], lhsT=sel, rhs=sm[:, 4:5], start=True, stop=True), reads=["sm4", "SelB"], writes=[("rps", 0)])
        P.op("act", lambda e, b=b: e.copy(out=psb_[b][:], in_=psr[0][:, 0:1]), reads=[("rps", 0)], writes=[("psb", b)])
        for sc in range(NSC):
            P.op("dve", lambda e, b=b, i=i, sc=sc: e.tensor_scalar(out=junk[:], in0=bcs[b][:], scalar1=psb_[b][:, 0:1], scalar2=rv[:, i * NSC + sc:i * NSC + sc + 1],
                                                                  op0=ALU.add, op1=ALU.is_le), reads=[("bcs", b), ("psb", b), "rv"], writes=["junk"])
            P.op("dve", lambda e, i=i, sc=sc: e.reduce_sum(out=cntf[:, i, sc:sc + 1], in_=junk[:], axis=AX.X), reads=["junk"], writes=["cntf"])
    P.op("dve", lambda e: e.tensor_scalar(out=cntf[:], in0=cntf[:], scalar1=float(T - 1), scalar2=None, op0=ALU.min), reads=["cntf"], writes=["cntf"])
    P.op("dve", lambda e: e.tensor_copy(out=idx_all[:], in_=cntf[:]), reads=["cntf"], writes=["idx_all"])


def emit_experts(nc, P, st, T, xn_d, modv2, g2n, ident, idx_all, ix, w1_d, b1_d, w2_d, b2_d, yb_d, ne, blk, nblk):
    sb = lambda name, shape, dt=F32: st.enter_context(nc.sbuf_tensor(name, shape, dt))
    NSC = blk // 128
    NF = DFF // 128
    scl = sb("x_scl", [128, 16])
    xg = [sb("x_xg%d" % i, [128, D]) for i in range(2)]
    xeT = sb("x_xeT", [128, 16, blk], BF16)
    w1g = [sb("x_w1g%d" % i, [128, 16, 512], BF16) for i in range(2)]
    w1u = [sb("x_w1u%d" % i, [128, 16, 512], BF16) for i in range(2)]
    w2b = [sb("x_w2b%d" % i, [128, NF, 512], BF16) for i in range(2)]
    b1t = [sb("x_b1t%d" % i, [128, 2 * NF]) for i in range(2)]
    b2t = [sb("x_b2t%d" % i, [128, D]) for i in range(2)]
    gt = [sb("x_gt%d" % i, [128, blk]) for i in range(2)]
    ut = [sb("x_ut%d" % i, [128, blk]) for i in range(2)]
    sg = [sb("x_sg%d" % i, [128, blk]) for i in range(2)]
    actT = sb("x_actT", [128, NF, blk], BF16)
    yst = [sb("x_yst%d" % i, [128, 512]) for i in range(2)]
    ps = [st.enter_context(nc.psum_tensor("x_ps%d" % i, [128, 512], F32)) for i in range(8)]
    P.op("dve", lambda e: e.scalar_tensor_tensor(out=scl[:], in0=modv2[:, 1, :], scalar=1.0, in1=g2n[:], op0=ALU.add, op1=ALU.mult),
         reads=["modv", "gv"], writes=["xscl"])
    cn = {"g": 0, "w": 0, "w1": 0, "w2": 0, "h": 0, "y": 0}
    IOA = bass.IndirectOffsetOnAxis
    for i in range(nblk):
        eb = i % 2
        P.op("pool", lambda e, i=i, eb=eb: e.indirect_dma_start(out=b1t[eb][:], out_offset=None, in_=b1_d[:, :], in_offset=IOA(ap=ix["b1"][:, i:i + 1], axis=0)),
             reads=["ix"], writes=[("b1t", eb)], dma=True)
        P.op("pool", lambda e, i=i, eb=eb: e.indirect_dma_start(out=b2t[eb][:], out_offset=None, in_=b2_d[:, :], in_offset=IOA(ap=ix["b2"][:, i:i + 1], axis=0)),
             reads=["ix"], writes=[("b2t", eb)], dma=True)
        for sc in range(NSC):
            gb = cn["g"] % 2
            cn["g"] += 1
            P.op("pool", lambda e, gb=gb, i=i, sc=sc: e.indirect_dma_start(out=xg[gb][:], out_offset=None, in_=xn_d[:, :],
                                                                          in_offset=bass.IndirectOffsetOnAxis(ap=idx_all[:, i, sc:sc + 1], axis=0)),
                 reads=["idx_all", "xn_all"], writes=[("xg", gb)], dma=True)
            for k in range(16):
                pb = gb * 4 + k // 4
                P.op("pe", lambda e, gb=gb, k=k, pb=pb: e.transpose(out=ps[pb][:, (k % 4) * 128:(k % 4 + 1) * 128], in_=xg[gb][:, k * 128:(k + 1) * 128], identity=ident[:]),
                     reads=[("xg", gb), "cst0"], writes=[("xps", pb)])
                P.op("dve" if k % 2 else "act", (lambda e, k=k, pb=pb, sc=sc: e.tensor_scalar(
                    out=xeT[:, k, sc * 128:(sc + 1) * 128], in0=ps[pb][:, (k % 4) * 128:(k % 4 + 1) * 128], scalar1=scl[:, k:k + 1], scalar2=modv2[:, 0, k:k + 1],
                    op0=ALU.mult, op1=ALU.add)) if k % 2 else (lambda e, k=k, pb=pb, sc=sc: e.activation(
                        out=xeT[:, k, sc * 128:(sc + 1) * 128], in_=ps[pb][:, (k % 4) * 128:(k % 4 + 1) * 128], func=AF.Identity, scale=scl[:, k:k + 1],
                        bias=modv2[:, 0, k:k + 1])),
                    reads=[("xps", pb), "xscl", "modv"], writes=["xeT"])
        for grp in range(3):
            wb_ = cn["w1"] % 2
            cn["w1"] += 1
            for (dst, pc) in ((w1g[wb_], grp * 2), (w1u[wb_], grp * 2 + 1)):
                P.op("pool", lambda e, dst=dst, pc=pc, i=i: e.indirect_dma_start(out=dst[:].rearrange("p k c -> p (k c)"), out_offset=None, in_=w1_d[:, :],
                                                                               in_offset=IOA(ap=ix["w1"][:, i, pc:pc + 1], axis=0)),
                     reads=["ix"], writes=[("w1", wb_)], dma=True)
            for j in range(4):
                fch = grp * 4 + j
                hb = cn["h"] % 2
                cn["h"] += 1
                for k in range(16):
                    P.op("pe", lambda e, k=k, j=j, wb_=wb_, hb=hb: e.matmul(ps[hb * 2][:, 0:blk], lhsT=w1g[wb_][:, k, j * 128:(j + 1) * 128], rhs=xeT[:, k, :],
                                                                           start=(k == 0), stop=(k == 15)), reads=[("w1", wb_), "xeT"], writes=[("xps", hb * 2)])
                for k in range(16):
                    P.op("pe", lambda e, k=k, j=j, wb_=wb_, hb=hb: e.matmul(ps[hb * 2 + 1][:, 0:blk], lhsT=w1u[wb_][:, k, j * 128:(j + 1) * 128], rhs=xeT[:, k, :],
                                                                           start=(k == 0), stop=(k == 15)), reads=[("w1", wb_), "xeT"], writes=[("xps", hb * 2 + 1)])
                P.op("act", lambda e, hb=hb, fch=fch, eb=eb: e.activation(out=gt[hb][:], in_=ps[hb * 2][:, 0:blk], func=AF.Identity, bias=b1t[eb][:, fch:fch + 1]),
                     reads=[("xps", hb * 2), ("b1t", eb)], writes=[("gt", hb)])
                P.op("dve", lambda e, hb=hb: e.tensor_scalar(out=gt[hb][:], in0=gt[hb][:], scalar1=7.0, scalar2=None, op0=ALU.min), reads=[("gt", hb)], writes=[("gt", hb)])
                P.op("act", lambda e, hb=hb: e.activation(out=sg[hb][:], in_=gt[hb][:], func=AF.Sigmoid, scale=1.702), reads=[("gt", hb)], writes=[("sg", hb)])
                P.op("dve", lambda e, hb=hb, fch=fch, eb=eb: e.tensor_scalar(out=ut[hb][:], in0=ps[hb * 2 + 1][:, 0:blk], scalar1=b1t[eb][:, NF + fch:NF + fch + 1], scalar2=None,
                                                                           op0=ALU.add), reads=[("xps", hb * 2 + 1), ("b1t", eb)], writes=[("ut", hb)])
                P.op("dve", lambda e, hb=hb: e.tensor_scalar(out=ut[hb][:], in0=ut[hb][:], scalar1=7.0, scalar2=-7.0, op0=ALU.min, op1=ALU.max), reads=[("ut", hb)],
                     writes=[("ut", hb)])
                P.op("dve", lambda e, hb=hb: e.scalar_tensor_tensor(out=ut[hb][:], in0=ut[hb][:], scalar=1.0, in1=gt[hb][:], op0=ALU.add, op1=ALU.mult),
                     reads=[("ut", hb), ("gt", hb)], writes=[("ut", hb)])
                P.op("dve", lambda e, hb=hb, fch=fch: e.tensor_tensor(out=actT[:, fch, :], in0=ut[hb][:], in1=sg[hb][:], op=ALU.mult),
                     reads=[("ut", hb), ("sg", hb)], writes=["actT"])
        for cg in range(4):
            wb_ = cn["w2"] % 2
            cn["w2"] += 1
            P.op("pool", lambda e, wb_=wb_, cg=cg, i=i: e.indirect_dma_start(out=w2b[wb_][:].rearrange("p k c -> p (k c)"), out_offset=None, in_=w2_d[:, :],
                                                                            in_offset=IOA(ap=ix["w2"][:, i, cg:cg + 1], axis=0)),
                 reads=["ix"], writes=[("w2", wb_)], dma=True)
            for sc in range(NSC):
                yb_ = cn["y"] % 2
                cn["y"] += 1
                pb = 4 + yb_
                for fk in range(NF):
                    P.op("pe", lambda e, fk=fk, sc=sc, wb_=wb_, pb=pb: e.matmul(ps[pb][:], lhsT=actT[:, fk, sc * 128:(sc + 1) * 128], rhs=w2b[wb_][:, fk, :],
                                                                               start=(fk == 0), stop=(fk == NF - 1)), reads=["actT", ("w2", wb_)], writes=[("xps", pb)])
                P.op("dve", lambda e, yb_=yb_, pb=pb, cg=cg, eb=eb: e.tensor_tensor(out=yst[yb_][:], in0=ps[pb][:], in1=b2t[eb][:, cg * 512:(cg + 1) * 512], op=ALU.add),
                     reads=[("xps", pb), ("b2t", eb)], writes=[("yst", yb_)])
                r0 = i * blk + sc * 128
                P.dma("act", yb_d[r0:r0 + 128, cg * 512:(cg + 1) * 512], yst[yb_][:], reads=[("yst", yb_)], writes=[("yb", i, sc, cg)])


def emit_combine(nc, P, st, T, x1_d, pm_d, slot_d, yb_d, g2b_d, out_d, ne):
    sb = lambda name, shape, dt=F32: st.enter_context(nc.sbuf_tensor(name, shape, dt))
    g2b = sb("o_g2b", [128, D])
    pmt = [sb("o_pm%d" % i, [128, ne]) for i in range(2)]
    slt = [sb("o_sl%d" % i, [128, ne]) for i in range(2)]
    oh = sb("o_oh", [128, ne])
    tmp = sb("o_tmp", [128, ne])
    vk = [sb("o_vk%d" % i, [128, 4]) for i in range(2)]
    sk = [sb("o_sk%d" % i, [128, 4]) for i in range(2)]
    ski = [sb("o_ski%d" % i, [128, 4], I32) for i in range(2)]
    rows = [sb("o_rows%d" % i, [128, D]) for i in range(3)]
    acc = [sb("o_acc%d" % i, [128, D]) for i in range(2)]
    xt = [sb("o_xt%d" % i, [128, D]) for i in range(2)]
    P.dma("sp", g2b[:], g2b_d, writes=["g2b"])
    rc = 0
    for t in range(T // 128):
        b = t % 2
        r0 = t * 128
        P.dma("sp", pmt[b][:], pm_d[r0:r0 + 128, :], reads=["pm_all"], writes=[("opm", b)])
        P.dma("sp", slt[b][:], slot_d[r0:r0 + 128, :], reads=["slot_all"], writes=[("osl", b)])
        P.dma("sp", xt[b][:], x1_d[r0:r0 + 128, :], reads=["x1_all"], writes=[("oxt", b)])
        for k in range(4):
            P.op("dve", lambda e, b=b, k=k: e.reduce_max(out=vk[b][:, k:k + 1], in_=pmt[b][:], axis=AX.X), reads=[("opm", b)], writes=[("vk", b)])
            P.op("dve", lambda e, b=b, k=k: e.tensor_scalar(out=oh[:], in0=pmt[b][:], scalar1=vk[b][:, k:k + 1], scalar2=None, op0=ALU.is_equal),
                 reads=[("opm", b), ("vk", b)], writes=["oh"])
            P.op("dve", lambda e, b=b: e.tensor_tensor(out=tmp[:], in0=oh[:], in1=slt[b][:], op=ALU.mult), reads=["oh", ("osl", b)], writes=["otmp"])
            P.op("dve", lambda e, b=b, k=k: e.reduce_sum(out=sk[b][:, k:k + 1], in_=tmp[:], axis=AX.X), reads=["otmp"], writes=[("sk", b)])
            P.op("dve", lambda e, b=b: e.tensor_tensor(out=tmp[:], in0=oh[:], in1=pmt[b][:], op=ALU.mult), reads=["oh", ("opm", b)], writes=["otmp"])
            P.op("dve", lambda e, b=b: e.tensor_tensor(out=pmt[b][:], in0=pmt[b][:], in1=tmp[:], op=ALU.subtract), reads=["otmp", ("opm", b)], writes=[("opm", b)])
        P.op("dve", lambda e, b=b: e.tensor_copy(out=ski[b][:], in_=sk[b][:]), reads=[("sk", b)], writes=[("ski", b)])
        for k in range(4):
            rb_ = rc % 3
            rc += 1
            P.op("pool", lambda e, b=b, k=k, rb_=rb_: e.indirect_dma_start(out=rows[rb_][:], out_offset=None, in_=yb_d[:, :],
                                                                         in_offset=bass.IndirectOffsetOnAxis(ap=ski[b][:, k:k + 1], axis=0)),
                 reads=[("ski", b), "yb_all"], writes=[("rows", rb_)], dma=True)
            if k == 0:
                P.op("dve", lambda e, b=b, rb_=rb_: e.tensor_scalar(out=acc[b][:], in0=rows[rb_][:], scalar1=vk[b][:, 0:1], scalar2=None, op0=ALU.mult),
                     reads=[("rows", rb_), ("vk", b)], writes=[("oacc", b)])
            else:
                P.op("dve", lambda e, b=b, k=k, rb_=rb_: e.scalar_tensor_tensor(out=acc[b][:], in0=rows[rb_][:], scalar=vk[b][:, k:k + 1], in1=acc[b][:],
                                                                              op0=ALU.mult, op1=ALU.add), reads=[("rows", rb_), ("vk", b), ("oacc", b)], writes=[("oacc", b)])
        P.op("pool", lambda e, b=b: e.tensor_tensor(out=acc[b][:], in0=acc[b][:], in1=g2b[:], op=ALU.mult), reads=[("oacc", b), "g2b"], writes=[("oacc", b)])
        P.op("pool", lambda e, b=b: e.tensor_tensor(out=xt[b][:], in0=xt[b][:], in1=acc[b][:], op=ALU.add), reads=[("oacc", b), ("oxt", b)], writes=[("oxt", b)])
        P.dma("sp", out_d[r0:r0 + 128, :], xt[b][:], reads=[("oxt", b)], writes=[("out", t)])


def build_lb(T, ne=NE, blk=512, debug=False, nph=99):
    nc = _new_nc()
    din = lambda name, shape, dt=F32: nc.dram_tensor(name, shape, dt, kind="ExternalInput").ap()
    NSC = blk // 128
    nblk = (T * 4) // blk + ne
    assert nblk <= 128
    x = din("x", [T, D])
    modv1_d = din("modv1", [128, 2, 16])
    g1n_d = din("g1n", [128, 16])
    modv2_d = din("modv2", [128, 2, 16])
    g2n_d = din("g2n", [128, 16])
    g1b_d = din("g1b", [128, D])
    g2b_d = din("g2b", [128, D])
    ident_d = din("ident", [128, 128])
    tri_d = din("tri", [128, 128])
    yaT_d = din("yaT", [1024, T])
    ybT_d = din("ybT", [1024, T])
    wgu_d = din("wgu", [128, 16, 1024])
    wgv_d = din("wgv", [128, 16, 1024])
    gd = dict(wsT=din("wsT", [128, 8, 128]), gbT=din("gbT", [128, 8, 128]), gng=din("gng", [128, 1024]))
    wg_f = din("wg_f", [16, 128, 3 * 16 * 128])
    wb_f = din("wb_f", [16, 128, 3 * 8 * 128])
    wo_f = din("wo_f", [16, 128, 2048])
    rd = dict(rw=din("rw", [128, 16, ne]), rb=din("rb", [128, ne]), bst=din("bst", [128, 1]), pidx=din("pidx", [ne, 1]), pcol=din("pcol", [128, 1]), thr=din("thr", [ne, T // blk]),
              rv=din("rv", [128, nblk * NSC]))
    w1_d = din("w1", [ne * 128 * 6, 16 * 512])
    b1_d = din("b1", [ne * 128, 24])
    w2_d = din("w2", [ne * 128 * 4, 12 * 512])
    b2_d = din("b2", [ne, D])
    kind = "ExternalOutput" if debug else "Internal"
    dscr = lambda name, shape, dt=F32, k=None: nc.dram_tensor(name, shape, dt, kind=(k or kind)).ap()
    hT_d = dscr("hT_d", [128, 16, T], BF16)
    zgu_d = dscr("zgu_d", [1024, T])
    zgv_d = dscr("zgv_d", [T, 1024])
    ycT_d = dscr("ycT_d", [128, 8, T], BF16)
    wg_b = dscr("wg_b", [16, 128, 3 * 16 * 128], BF16, "Internal")
    wb_b = dscr("wb_b", [16, 128, 3 * 8 * 128], BF16, "Internal")
    wo_b = dscr("wo_b", [16, 128, 2048], BF16, "Internal")
    x1_d = dscr("x1_d", [T, D])
    h2T_d = dscr("h2T_d", [128, 16, T], BF16)
    xn_d = dscr("xn_d", [T, D])
    pm_d = dscr("pm_d", [T, ne])
    slot_d = dscr("slot_d", [T, ne])
    yb_d = dscr("yb_d", [nblk * blk, D])
    out_d = nc.dram_tensor("out", [T, D], F32, kind="ExternalOutput").ap()
    cnt_d = nc.dram_tensor("cnt", [ne, 1], F32, kind="ExternalOutput").ap()
    with contextlib.ExitStack() as st0:
        P = Prog(nc, st0)
        sb0 = lambda name, shape, dt=F32: st0.enter_context(nc.sbuf_tensor(name, shape, dt))
        ident = sb0("ident_s", [128, 128])
        tri = sb0("tri_s", [128, 128])
        modv1 = sb0("modv1_s", [128, 2, 16])
        g1n = sb0("g1n_s", [128, 16])
        modv2 = sb0("modv2_s", [128, 2, 16])
        g2n = sb0("g2n_s", [128, 16])
        epst = sb0("eps_s", [128, 1])
        idx_all = sb0("idx_all", [128, nblk, NSC], I32)
        ixt = dict(w1=sb0("ix_w1", [128, nblk, 6], I32), w2=sb0("ix_w2", [128, nblk, 4], I32), b1=sb0("ix_b1", [128, nblk], I32),
                   b2=sb0("ix_b2", [128, nblk], I32))
        ones = sb0("ones_s", [128, 128])
        P.dma("sp", ident[:], ident_d, writes=["a"])
        P.dma("sp", tri[:], tri_d, writes=["b"])
        P.dma("sp", modv1[:], modv1_d, writes=["c"])
        P.dma("sp", g1n[:], g1n_d, writes=["d"])
        P.dma("sp", modv2[:], modv2_d, writes=["e"])
        P.dma("sp", g2n[:], g2n_d, writes=["f"])
        P.op("dve", lambda e: e.memset(epst[:], EPS), writes=["g"])
        P.op("dve", lambda e: e.memset(ones[:], 1.0), writes=["h"])
        P.flush()
        ph = 1
        if nph >= 1:
          with contextlib.ExitStack() as st:
            emit_norm_to_hT(nc, P, st, x, T, modv1, g1n, ident, epst, hT_d)
            P.flush()
        if nph >= 2:
          with contextlib.ExitStack() as st:
            emit_proj(nc, P, st, T, hT_d, wgu_d, 1024, True, zgu_d, "pu")
            P.flush()
          with contextlib.ExitStack() as st:
            emit_proj(nc, P, st, T, hT_d, wgv_d, 1024, False, zgv_d, "pv")
            P.flush()
        if nph >= 3:
          with contextlib.ExitStack() as st:
            emit_gmlp(nc, P, st, T, zgu_d, zgv_d, ycT_d, gd, tri, ident, epst)
            P.flush()
        if nph >= 4:
          with contextlib.ExitStack() as st:
            emit_cast_to_dram(nc, P, st, wg_f, wg_b, 16, 3 * 16 * 128, "cg")
            P.flush()
          with contextlib.ExitStack() as st:
            emit_cast_to_dram(nc, P, st, wb_f, wb_b, 16, 3 * 8 * 128, "cb")
            emit_cast_to_dram(nc, P, st, wo_f, wo_b, 16, 2048, "co")
            P.flush()
          with contextlib.ExitStack() as st:
            emit_merge(nc, P, st, T, x, hT_d, yaT_d, ybT_d, ycT_d,
                       wg_b.rearrange("f p (i k c) -> f p i k c", i=3, k=16), wb_b.rearrange("f p (i k c) -> f p i k c", i=3, k=8),
                       wo_b.rearrange("k p n -> p k n"), g1b_d, x1_d)
            P.flush()
        if nph >= 5:
          with contextlib.ExitStack() as st:
            emit_route(nc, P, st, T, x1_d, modv2, g2n, ident, tri, ones, epst, h2T_d, xn_d, rd, pm_d, slot_d, idx_all, ixt, cnt_d, ne, blk, nblk)
            P.flush()
        if nph >= 6:
          with contextlib.ExitStack() as st:
            emit_experts(nc, P, st, T, xn_d, modv2, g2n, ident, idx_all, ixt, w1_d, b1_d, w2_d, b2_d, yb_d, ne, blk, nblk)
            P.flush()
        if nph >= 7:
          with contextlib.ExitStack() as st:
            emit_combine(nc, P, st, T, x1_d, pm_d, slot_d, yb_d, g2b_d, out_d, ne)
            P.flush()
    return nc


def lb_inputs(l, b, T, x_rows, yaT, ybT, mod, inp, ne=NE, blk=512):
    m = mod[l, b]
    sh1, sc1, g1, sh2, sc2, g2 = [m[i * D:(i + 1) * D] for i in range(6)]
    pk = lambda v: np.ascontiguousarray(v.reshape(16, 128).T)
    rep = lambda v: np.ascontiguousarray(np.broadcast_to(v[None, :], (128, len(v))))
    w_in = inp["w_in"][l]
    wl = lambda w: np.ascontiguousarray(w.reshape(-1, 128, w.shape[1]).transpose(1, 0, 2))
    wgate = inp["w_gate"][l]
    wbr = inp["w_branch"][l]
    wg_f = np.ascontiguousarray(wgate.reshape(3, 16, 128, 16, 128).transpose(3, 2, 0, 1, 4).reshape(16, 128, 3 * 16 * 128))
    wb_f = np.ascontiguousarray(wbr.reshape(3, 8, 128, 16, 128).transpose(3, 2, 0, 1, 4).reshape(16, 128, 3 * 8 * 128))
    wo_f = np.ascontiguousarray(inp["w_out"][l].reshape(16, 128, D))
    ws = inp["gmlp_ws"][l]
    NSC = blk // 128
    nblk = (T * 4) // blk + ne
    d = {
        "x": np.ascontiguousarray(x_rows),
        "modv1": np.ascontiguousarray(np.stack([pk(sh1), pk(sc1)], axis=1)), "g1n": pk(inp["norm1_g"][l]),
        "modv2": np.ascontiguousarray(np.stack([pk(sh2), pk(sc2)], axis=1)), "g2n": pk(inp["norm2_g"][l]),
        "g1b": rep(g1), "g2b": rep(g2),
        "ident": np.eye(128, dtype=np.float32), "tri": np.triu(np.ones((128, 128), np.float32)),
        "yaT": np.ascontiguousarray(yaT), "ybT": np.ascontiguousarray(ybT),
        "wgu": wl(w_in[:, OFF["gu"]:OFF["gu"] + 1024]), "wgv": wl(w_in[:, OFF["gv"]:OFF["gv"] + 1024]),
        "wsT": np.ascontiguousarray(ws.transpose(2, 0, 1)),
        "gbT": np.ascontiguousarray(np.broadcast_to(inp["gmlp_b"][l][None], (128, 8, 128))),
        "gng": rep(inp["gmlp_norm_g"][l]),
        "wg_f": wg_f, "wb_f": wb_f, "wo_f": wo_f,
        "rw": wl(inp["router_w"][l][:, :ne]), "rb": rep(inp["router_b"][l][:ne]),
        "bst": np.ascontiguousarray((np.arange(128, dtype=np.float32) * blk).reshape(128, 1)),
        "pidx": np.ascontiguousarray(np.arange(ne, dtype=np.float32).reshape(ne, 1)),
        "rv": np.ascontiguousarray((np.arange(nblk * NSC)[None, :] * 128 + np.arange(128)[:, None]).astype(np.float32)),
        "w1": np.ascontiguousarray(inp["exp_w1"][l][:ne].reshape(ne, 16, 128, 2, 3, 512).transpose(0, 2, 4, 3, 1, 5)).reshape(ne * 128 * 6, 16 * 512),
        "b1": np.ascontiguousarray(inp["exp_b1"][l][:ne].reshape(ne, 24, 128).transpose(0, 2, 1)).reshape(ne * 128, 24),
        "w2": np.ascontiguousarray(inp["exp_w2"][l][:ne].reshape(ne, 12, 128, 4, 512).transpose(0, 2, 3, 1, 4)).reshape(ne * 128 * 4, 12 * 512),
        "b2": np.ascontiguousarray(inp["exp_b2"][l][:ne]),
        "pcol": np.arange(128, dtype=np.float32).reshape(128, 1),
        "thr": np.ascontiguousarray(np.broadcast_to((np.arange(T // blk, dtype=np.float32) * blk)[None, :], (ne, T // blk))),
    }
    return d


_CACHE = {}


def _get(name, fn):
    if name not in _CACHE:
        _CACHE[name] = fn()
    return _CACHE[name]


def kernel(**inputs):
    inp = {k: np.asarray(v) for k, v in inputs.items()}
    x = np.ascontiguousarray(inp["x"], dtype=np.float32)
    mod = run_ada(inp["c"], inp["ada_w"], inp["ada_b"])
    TB = SEQ // 2
    for l in range(DEPTH):
        ncA = _get("la", lambda: build_la(SEQ))
        in_maps = [la_inputs(l, b, hh, SEQ, x[b], mod, inp) for b in range(NB) for hh in range(2)]
        resA = run_bass_kernel_spmd(ncA, in_maps, core_ids=list(range(8)))
        ya = np.empty((NB, SEQ, 1024), np.float32)
        yb = np.empty((NB, SEQ, 1024), np.float32)
        for b in range(NB):
            for hh in range(2):
                r = resA.results[b * 2 + hh]
                ya[b, :, hh * 512:(hh + 1) * 512] = r["ya"]
                yb[b, :, hh * 512:(hh + 1) * 512] = r["yb"]
        del resA, in_maps
        ncB = _get("lb", lambda: build_lb(TB))
        shared = None
        in_maps = []
        for core in range(8):
            b, half = core // 2, core % 2
            rows = slice(half * TB, (half + 1) * TB)
            d = lb_inputs(l, b, TB, x[b, rows], ya[b, rows].T, yb[b, rows].T, mod, inp) if shared is None else \
                lb_inputs_light(l, b, TB, x[b, rows], ya[b, rows].T, yb[b, rows].T, mod, inp, shared)
            if shared is None:
                shared = d
            in_maps.append(d)
        resB = run_bass_kernel_spmd(ncB, in_maps, core_ids=list(range(8)))
        xn = np.empty_like(x)
        for core in range(8):
            b, half = core // 2, core % 2
            xn[b, half * TB:(half + 1) * TB] = resB.results[core]["out"]
        _CACHE["cnt_%d" % l] = np.stack([resB.results[core]["cnt"].ravel() for core in range(8)])
        x = xn
        del resB, in_maps, shared
    return x


_SHARED_KEYS = ("ident", "tri", "wgu", "wgv", "wsT", "gbT", "gng", "wg_f", "wb_f", "wo_f", "rw", "rb", "bst", "pidx", "rv", "pcol", "thr",
                "w1", "b1", "w2", "b2", "g1n", "g2n")


def lb_inputs_light(l, b, T, x_rows, yaT, ybT, mod, inp, shared):
    m = mod[l, b]
    sh1, sc1, g1, sh2, sc2, g2 = [m[i * D:(i + 1) * D] for i in range(6)]
    pk = lambda v: np.ascontiguousarray(v.reshape(16, 128).T)
    rep = lambda v: np.ascontiguousarray(np.broadcast_to(v[None, :], (128, len(v))))
    d = {k: shared[k] for k in _SHARED_KEYS}
    d.update({
        "x": np.ascontiguousarray(x_rows),
        "modv1": np.ascontiguousarray(np.stack([pk(sh1), pk(sc1)], axis=1)),
        "modv2": np.ascontiguousarray(np.stack([pk(sh2), pk(sc2)], axis=1)),
        "g1b": rep(g1), "g2b": rep(g2),
        "yaT": np.ascontiguousarray(yaT), "ybT": np.ascontiguousarray(ybT),
    })
    return d
```
